# Optimizing a Trainium2 kernel written in Bass

```python
import math
import jax, jax.numpy as jnp
from jax import lax
import numpy as np

D_MODEL = 1024
BATCH = 16
SEQ = 4096
DEPTH = 4

HEAD_DIM = 64
N_HEADS_A = 8
DILATION_PATTERNS = ((128, 1), (512, 4), (2048, 16))
N_HEADS_B = 8
N_KV_B = 2
WINDOW_B = 128
WA = N_HEADS_A * HEAD_DIM
WB_Q = N_HEADS_B * HEAD_DIM
WB_KV = N_KV_B * HEAD_DIM
MIX_WIDTH = WA + WB_Q
IN_WIDTH = 3 * WA + WB_Q + 2 * WB_KV
ROPE_THETA = 10000.0
N_EXPERTS = 16
D_EXPERT = 1024
CAPACITY_FACTOR = 2
RMS_EPS = 1e-6
NEG_INF = -1e30

kernel_name = "hybrid_longnet_swa_sink_ec_moe_encoder"


def rmsnorm(x, g):
    xf = x.astype(jnp.float32)
    y = xf * lax.rsqrt(jnp.mean(xf * xf, axis=-1, keepdims=True) + RMS_EPS)
    return (y * g.astype(jnp.float32)).astype(x.dtype)


def rope_tables(seq_len):
    inv = 1.0 / (ROPE_THETA ** (jnp.arange(0, HEAD_DIM, 2, dtype=jnp.float32) / HEAD_DIM))
    ang = jnp.arange(seq_len, dtype=jnp.float32)[:, None] * inv[None, :]
    return jnp.cos(ang), jnp.sin(ang)


def apply_rope(t, cos, sin):
    tf = t.astype(jnp.float32)
    t1, t2 = tf[..., : HEAD_DIM // 2], tf[..., HEAD_DIM // 2:]
    out = jnp.concatenate([t1 * cos - t2 * sin, t2 * cos + t1 * sin], axis=-1)
    return out.astype(t.dtype)


def banded_attention(q, k, v, half_window, sink=None):
    w = half_window
    L, hd = q.shape[-2], q.shape[-1]
    nb = -(-L // w)
    pad = nb * w - L
    qp = jnp.pad(q, [(0, 0)] * (q.ndim - 2) + [(0, pad), (0, 0)])
    qb = qp.reshape(q.shape[:-2] + (nb, w, hd))

    def windows(t):
        tp = jnp.pad(t, [(0, 0)] * (t.ndim - 2) + [(w, pad + w), (0, 0)])
        tb = tp.reshape(t.shape[:-2] + (nb + 2, w, hd))
        return jnp.concatenate([tb[..., 0:nb, :, :], tb[..., 1:nb + 1, :, :], tb[..., 2:nb + 2, :, :]], axis=-2)

    kw, vw = windows(k), windows(v)
    s = jnp.einsum('...gbqd,...bkd->...gbqk', qb, kw, preferred_element_type=jnp.float32) * (hd ** -0.5)
    qpos = jnp.arange(nb)[:, None] * w + jnp.arange(w)[None, :]
    kpos = (jnp.arange(nb)[:, None] - 1) * w + jnp.arange(3 * w)[None, :]
    valid = ((jnp.abs(qpos[:, :, None] - kpos[:, None, :]) <= w)
             & (kpos[:, None, :] >= 0) & (kpos[:, None, :] < L))
    s = jnp.where(valid, s, NEG_INF)
    m = jnp.max(s, axis=-1)
    if sink is not None:
        sink_b = jnp.broadcast_to(sink.astype(jnp.float32), m.shape)
        m = jnp.maximum(m, sink_b)
    e = jnp.exp(s - m[..., None])
    denom = jnp.sum(e, axis=-1)
    if sink is not None:
        denom = denom + jnp.exp(sink_b - m)
    p = e / denom[..., None]
    o = jnp.einsum('...gbqk,...bkd->...gbqd', p.astype(v.dtype), vw)
    lse = m + jnp.log(denom)
    o = o.reshape(o.shape[:-4] + (o.shape[-4], nb * w, hd))[..., :L, :]
    lse = lse.reshape(lse.shape[:-2] + (nb * w,))[..., :L]
    return o, lse


def longnet_dilated_attention(q, k, v):
    B, H, S, hd = q.shape
    outs, lses = [], []
    for window, d in DILATION_PATTERNS:
        n = S // d
        qd = q.reshape(B, H, n, d, hd).swapaxes(2, 3)[:, :, :, None]
        kd = k.reshape(B, H, n, d, hd).swapaxes(2, 3)
        vd = v.reshape(B, H, n, d, hd).swapaxes(2, 3)
        o, lse = banded_attention(qd, kd, vd, (window // 2) // d)
        outs.append(o[:, :, :, 0].swapaxes(2, 3).reshape(B, H, S, hd))
        lses.append(lse[:, :, :, 0].swapaxes(2, 3).reshape(B, H, S))
    wts = jax.nn.softmax(jnp.stack(lses, axis=0), axis=0)
    out = sum(wts[i][..., None] * outs[i].astype(jnp.float32) for i in range(len(outs)))
    return out.astype(q.dtype)


def expert_choice_moe(h, w_router, w_gate, w_up, w_down):
    B, S, D = h.shape
    C = min(CAPACITY_FACTOR * S // N_EXPERTS, S)
    logits = jnp.einsum('bsd,de->bse', h, w_router, preferred_element_type=jnp.float32)
    aff = jax.nn.softmax(logits, axis=-1)
    gate, idx = lax.top_k(jnp.swapaxes(aff, 1, 2), C)
    bidx = jnp.arange(B)[:, None, None]
    xe = h[bidx, idx]
    hid = jax.nn.silu(jnp.einsum('becd,edf->becf', xe, w_gate)) * jnp.einsum('becd,edf->becf', xe, w_up)
    ye = jnp.einsum('becf,efd->becd', hid, w_down) * gate[..., None].astype(h.dtype)
    return jnp.zeros_like(h).at[bidx, idx].add(ye)


def setup_inputs(seed: int = 0) -> dict:
    key = jax.random.key(seed)
    ks = jax.random.split(key, 16)
    f32 = jnp.float32
    nrm = lambda k, shape, scale: jax.random.normal(k, shape, f32) * scale
    gain = lambda k, shape: 1.0 + 0.02 * jax.random.normal(k, shape, f32)
    return {
        "x": jax.random.normal(ks[0], (BATCH, SEQ, D_MODEL), f32),
        "w_in": nrm(ks[1], (DEPTH, D_MODEL, IN_WIDTH), D_MODEL ** -0.5),
        "w_out": nrm(ks[2], (DEPTH, MIX_WIDTH, D_MODEL), MIX_WIDTH ** -0.5),
        "g_attn": gain(ks[3], (DEPTH, D_MODEL)),
        "g_mix_a": gain(ks[4], (DEPTH, WA)),
        "g_mix_b": gain(ks[5], (DEPTH, WB_Q)),
        "sink": nrm(ks[6], (DEPTH, N_KV_B, N_HEADS_B // N_KV_B), 1.0),
        "g_ffn": gain(ks[7], (DEPTH, D_MODEL)),
        "w_router": nrm(ks[8], (DEPTH, D_MODEL, N_EXPERTS), D_MODEL ** -0.5),
        "w_gate": nrm(ks[9], (DEPTH, N_EXPERTS, D_MODEL, D_EXPERT), D_MODEL ** -0.5),
        "w_up": nrm(ks[10], (DEPTH, N_EXPERTS, D_MODEL, D_EXPERT), D_MODEL ** -0.5),
        "w_down": nrm(ks[11], (DEPTH, N_EXPERTS, D_EXPERT, D_MODEL), D_EXPERT ** -0.5),
        "g_final": gain(ks[12], (D_MODEL,)),
    }


def reference(x, w_in, w_out, g_attn, g_mix_a, g_mix_b, sink, g_ffn,
              w_router, w_gate, w_up, w_down, g_final):
    B, S, D = x.shape
    cos, sin = rope_tables(S)
    split_pts = np.cumsum([WA, WA, WA, WB_Q, WB_KV])

    def heads(t, n):
        return t.reshape(B, S, n, HEAD_DIM).transpose(0, 2, 1, 3)

    for l in range(DEPTH):
        h = rmsnorm(x, g_attn[l])
        proj = jnp.einsum('bsd,de->bse', h, w_in[l])
        qa, ka, va, qb, kb, vb = jnp.split(proj, split_pts, axis=-1)
        qa = apply_rope(heads(qa, N_HEADS_A), cos, sin)
        ka = apply_rope(heads(ka, N_HEADS_A), cos, sin)
        oa = longnet_dilated_attention(qa, ka, heads(va, N_HEADS_A))
        oa = oa.transpose(0, 2, 1, 3).reshape(B, S, WA)
        g = N_HEADS_B // N_KV_B
        qb = apply_rope(heads(qb, N_HEADS_B), cos, sin).reshape(B, N_KV_B, g, S, HEAD_DIM)
        kb = apply_rope(heads(kb, N_KV_B), cos, sin)
        ob, _ = banded_attention(qb, kb, heads(vb, N_KV_B), WINDOW_B, sink=sink[l][:, :, None, None])
        ob = ob.reshape(B, N_HEADS_B, S, HEAD_DIM).transpose(0, 2, 1, 3).reshape(B, S, WB_Q)
        mix = jnp.concatenate([rmsnorm(oa, g_mix_a[l]), rmsnorm(ob, g_mix_b[l])], axis=-1)
        x = x + jnp.einsum('bse,ed->bsd', mix, w_out[l])
        h2 = rmsnorm(x, g_ffn[l])
        x = x + expert_choice_moe(h2, w_router[l], w_gate[l], w_up[l], w_down[l])
    return rmsnorm(x, g_final)
```

```python
import numpy as np
import ml_dtypes
from contextlib import ExitStack
import concourse.bass as bass
import concourse.mybir as mybir
from concourse.bass_utils import run_bass_kernel_spmd

F32 = mybir.dt.float32
BF = mybir.dt.bfloat16
U32 = mybir.dt.uint32
I32 = mybir.dt.int32
AF = mybir.ActivationFunctionType
ALU = mybir.AluOpType
AX = mybir.AxisListType

D = 1024
HD = 64
INW = 2304
NE = 16
EPS = 1e-6
NEG = -30000.0
PATTERNS = (1, 4, 16)
PAIRCOL = [0, 128, 256, 384, 512, 640, 768, 896, 1536, 1664, 1792, 1920, 2048]


class Buf:
    __slots__ = ("w", "r", "name")

    def __init__(self, name):
        self.name = name
        self.w = {}
        self.r = {}


class KB:
    ROLL = 16000

    def __init__(self, nc, es):
        self.nc = nc
        self.es = es
        self.E = dict(pe=nc.tensor, act=nc.scalar, dve=nc.vector, pool=nc.gpsimd, sp=nc.sync)
        self.nsem = 0
        self.esem = {e: self.newsem("e_" + e) for e in self.E}
        self.waited = {e: {} for e in self.E}
        self.dsems = []
        self.dcache = {}
        self.esems_all = list(self.esem.values())

    def newsem(self, name):
        self.nsem += 1
        h = self.es.enter_context(self.nc.semaphore("%s_%d" % (name, self.nsem)))
        return [h, 0]

    def dsem(self, name, idx=0):
        key = (name, idx)
        if key not in self.dcache:
            s = self.newsem(name)
            self.dsems.append(s)
            self.dcache[key] = s
        return self.dcache[key]

    def _wait(self, eng, toks):
        need = {}
        for s, v in toks:
            k = id(s)
            if k not in need or need[k][1] < v:
                need[k] = (s, v)
        wd = self.waited[eng]
        for k, (s, v) in need.items():
            if wd.get(k, 0) < v:
                self.E[eng].wait_ge(s[0], v)
                wd[k] = v

    def pre(self, eng, R, W, Wd):
        toks = []
        for b in R:
            toks.extend(b.w.values())
        for b in W:
            toks.extend(b.w.values())
            toks.extend(b.r.values())
        for b in Wd:
            toks.extend(b.r.values())
        self._wait(eng, toks)

    def post(self, tok, R, W, Wd):
        s, v = tok
        k = id(s)
        for b in R:
            b.r[k] = tok
        for b in W:
            b.w[k] = tok
        for b in Wd:
            b.w[k] = tok

    def op(self, eng, fn, R=(), W=(), Wd=()):
        self.pre(eng, R, W, Wd)
        ins = fn(self.E[eng])
        if isinstance(ins, (list, tuple)):
            ins = ins[-1]
        s = self.esem[eng]
        if s[1] >= self.ROLL:
            s = self.esem[eng] = self.newsem("e_" + eng)
            self.esems_all.append(s)
        s[1] += 1
        ins.then_inc(s[0], 1)
        self.post((s, s[1]), R, W, Wd)

    def dma(self, q, sem, out, in_, R=(), W=(), Wd=()):
        self.pre(q, R, W, Wd)
        ins = self.E[q].dma_start(out=out, in_=in_)
        sem[1] += 16
        ins.then_inc(sem[0], 16)
        self.post((sem, sem[1]), R, W, Wd)

    def idma(self, sem, R=(), W=(), Wd=(), **kw):
        self.pre("pool", R, W, Wd)
        ins = self.nc.gpsimd.indirect_dma_start(**kw)
        sem[1] += 16
        ins.then_inc(sem[0], 16)
        self.post((sem, sem[1]), R, W, Wd)

    def barrier(self):
        toks = [(s, s[1]) for s in self.esems_all if s[1] > 0]
        toks += [(s, s[1]) for s in self.dsems if s[1] > 0]
        for e in self.E:
            self._wait(e, toks)


def build_program(S, NS, L, dbg=False, skip=()):
    C = 2 * S // NE
    NT = S // 128
    NG = S // 512
    NCC = C // 128
    nc = bass.Bass("TRN2", target_bir_lowering=False)

    def din(name, shape, dt):
        return nc.dram_tensor(name, list(shape), dt, kind="ExternalInput").ap()

    x_in = din("x", [NS, S, D], F32)
    w_in = din("w_in", [L, D, INW], F32)
    w_out = din("w_out", [L, D, D], F32)
    g_attn = din("g_attn", [L, D], F32)
    g_mix = din("g_mix", [L, D], F32)
    sink = din("sink", [L, 8], F32)
    g_ffn = din("g_ffn", [L, D], F32)
    w_router = din("w_router", [L, D, NE], F32)
    w_gate = din("w_gate", [L, NE, D, D], F32)
    w_up = din("w_up", [L, NE, D, D], F32)
    w_down = din("w_down", [L, NE, D, D], F32)
    g_final = din("g_final", [1, D], F32)
    cos_d = din("cosT", [128, S], F32)
    sin_d = din("sinT", [128, S], F32)
    identb_d = din("ident_bf", [128, 128], BF)
    identf_d = din("ident_f", [128, 128], F32)
    maskA_d = din("maskA", [128, 256], BF)
    maskB_d = din("maskB", [128, 384], BF)
    y_out = nc.dram_tensor("y", [NS, S, D], F32, kind="ExternalOutput").ap()

    skind = "ExternalOutput" if dbg else "Internal"

    def dscr(name, shape, dt):
        return nc.dram_tensor(name, list(shape), dt, kind=skind).ap()

    xr = [dscr("xr%d" % i, [S, D], F32) for i in range(NS)]
    QT = dscr("QT", [NS, 13, 128, S], BF)
    Vd = dscr("Vd", [NS, S, 640], BF)
    mixT = dscr("mixT", [NS, D, S], BF)
    h2d = [dscr("h2d%d" % i, [S, D], BF) for i in range(NS)]
    if dbg:
        dbg_idx = dscr("dbg_idx", [128, NCC, 48], I32)
        dbg_gate = dscr("dbg_gate", [128, NCC, 48], F32)
        dbg_aff = dscr("dbg_aff", [128, S], F32)

    with ExitStack() as es:
        k = KB(nc, es)

        uid = [0]

        def sbt(st, name, shape, dt):
            uid[0] += 1
            return st.enter_context(nc.sbuf_tensor("%s_%d" % (name, uid[0]), list(shape), dt))

        def pst(st, name, shape, dt):
            uid[0] += 1
            return st.enter_context(nc.psum_tensor("%s_%d" % (name, uid[0]), list(shape), dt))

        b_xr = [Buf("xr%d" % s) for s in range(NS)]
        b_QT = [Buf("QT%d" % s) for s in range(NS)]
        b_Vd = [Buf("Vd%d" % s) for s in range(NS)]
        b_mixT = [Buf("mixT%d" % s) for s in range(NS)]
        b_h2d = [Buf("h2d%d" % s) for s in range(NS)]
        b_y = Buf("y")

        identb = sbt(es, "identb", [128, 128], BF)
        identf = sbt(es, "identf", [128, 128], F32)
        maskA = sbt(es, "maskA_s", [128, 256], BF)
        maskB = sbt(es, "maskB_s", [128, 384], BF)
        onesf = sbt(es, "onesf", [128, 64], F32)
        epst = sbt(es, "epst", [128, 1], F32)
        affT = sbt(es, "affT", [128, S], F32)
        idxT = sbt(es, "idxT", [128, NCC, 48], I32)
        gateT = sbt(es, "gateT", [128, NCC, 48], F32)
        b_const = Buf("const")
        b_affT = Buf("affT")
        b_idxT = Buf("idxT")
        b_gateT = Buf("gateT")
        csem = k.dsem("csem")
        k.dma("sp", csem, identb[:], identb_d, W=[b_const])
        k.dma("sp", csem, identf[:], identf_d, Wd=[b_const])
        k.dma("sp", csem, maskA[:], maskA_d, Wd=[b_const])
        k.dma("sp", csem, maskB[:], maskB_d, Wd=[b_const])
        k.op("dve", lambda e: e.memset(onesf[:], 1.0), Wd=[b_const])
        onesbp = sbt(es, "onesbp", [128, 64], BF)
        k.op("dve", lambda e: e.memset(onesbp[:], 1.0), Wd=[b_const])
        k.op("dve", lambda e: e.memset(epst[:], EPS), Wd=[b_const])
        k.op("dve", lambda e: e.memset(affT[:], 0.0), W=[b_affT])
        mA01 = sbt(es, "mA01", [128, 256], BF)
        mB01 = sbt(es, "mB01", [128, 384], BF)
        b_m01 = Buf("m01")
        k.op("dve", lambda e: e.tensor_scalar(out=mA01[:], in0=maskA[:], scalar1=0.0, scalar2=None,
                                              op0=ALU.is_equal), R=[b_const], W=[b_m01])
        k.op("dve", lambda e: e.tensor_scalar(out=mB01[:], in0=maskB[:], scalar1=0.0, scalar2=None,
                                              op0=ALU.is_equal), R=[b_const], Wd=[b_m01])

        def rms_rstd(st_tiles, xt, b_xt, width, ssq, rt, rstd, junk, b_t):
            k.op("act", lambda e: e.activation(out=junk, in_=xt, func=AF.Square, accum_out=ssq),
                 R=[b_xt], W=[b_t])
            k.op("act", lambda e: e.activation(out=rt, in_=ssq, func=AF.Sqrt, bias=epst[:, 0:1],
                                               scale=1.0 / width), R=[b_t, b_const], W=[b_t])
            k.op("dve", lambda e: e.reciprocal(out=rstd, in_=rt), R=[b_t], W=[b_t])

        for l in range(L):
            xsrc, b_xsrc = ([x_in[i] for i in range(NS)], None) if l == 0 else (xr, b_xr)

            with ExitStack() as ph:
                Wbf = sbt(ph, "Wbf", [128, 8, INW], BF)
                Wrot = sbt(ph, "Wrot", [128, 8, INW], BF)
                gA = sbt(ph, "gA", [128, D], F32)
                b_W = Buf("Wbf")
                b_Wrot = Buf("Wrot")
                b_gA = Buf("gA")
                wsem = k.dsem("wsemA")
                wv = w_in[l].rearrange("(c p) n -> p c n", p=128)
                for c0 in range(0, INW, 1152):
                    k.dma("pool", wsem, Wbf[:, :, c0:c0 + 1152], wv[:, :, c0:c0 + 1152], Wd=[b_W])
                k.dma("sp", k.dsem("gAsem"), gA[:], g_attn[l:l + 1, :].partition_broadcast(128), W=[b_gA])
                for c in range(8):
                    for (c0, nh) in ((0, 16), (1536, 10)):
                        src = Wbf[:, c, c0:c0 + nh * 64].rearrange("p (h t i) -> p h t i", t=2, i=32)
                        dst = Wrot[:, c, c0:c0 + nh * 64].rearrange("p (h t i) -> p h t i", t=2, i=32)
                        k.op("act", lambda e, s_=src, d_=dst: e.mul(
                            out=d_[:, :, 0, :], in_=s_[:, :, 1, :], mul=-1.0), R=[b_W], Wd=[b_Wrot])
                        k.op("dve", lambda e, s_=src, d_=dst: e.tensor_copy(
                            out=d_[:, :, 1, :], in_=s_[:, :, 0, :]), R=[b_W], Wd=[b_Wrot])

                NB = 2
                xt = [sbt(ph, "xtA%d" % i, [128, D], F32) for i in range(4)]
                junk = [sbt(ph, "junkA%d" % i, [128, D], BF) for i in range(4)]
                hb = [sbt(ph, "hbA%d" % i, [128, D], BF) for i in range(4)]
                st3 = [sbt(ph, "stA%d" % i, [128, 4], F32) for i in range(4)]
                hT = [sbt(ph, "hT%d" % i, [128, 8, 512], BF) for i in range(NB)]
                vst = [sbt(ph, "vst%d" % i, [128, 640], BF) for i in range(NB)]
                cs = [sbt(ph, "cs%d" % i, [128, 2, 512], F32) for i in range(NB)]
                t12 = [sbt(ph, "t12_%d" % i, [128, 2, 512], F32) for i in range(NB)]
                qk = [sbt(ph, "qk%d" % i, [128, 512], BF) for i in range(NB)]
                tp_ps = [pst(ph, "tp_psA%d" % i, [128, D], BF) for i in range(2)]
                v_ps = pst(ph, "v_psA", [128, 512], F32)
                vb_ps = pst(ph, "vb_psA", [128, 512], F32)
                x_ps = [pst(ph, "x_psA%d" % i, [128, 512], F32) for i in range(NB)]
                r_ps = [pst(ph, "r_psA%d" % i, [128, 512], F32) for i in range(NB)]
                b_xt = [Buf("xt") for _ in range(4)]
                b_st = [Buf("st") for _ in range(4)]
                b_hb = [Buf("hb") for _ in range(4)]
                b_hT = [Buf("hT") for _ in range(NB)]
                b_vst = [Buf("vst") for _ in range(NB)]
                b_cs = [Buf("cs") for _ in range(NB)]
                b_t12 = [Buf("t12") for _ in range(NB)]
                b_qk = [Buf("qk") for _ in range(NB)]
                b_tp = [Buf("tp") for _ in range(2)]
                b_vps = Buf("vps")
                b_xps = [Buf("xps") for _ in range(NB)]
                s_xt = [k.dsem("s_xt", _i) for _i in range(4)]
                s_vst = [k.dsem("s_vst", _i) for _i in range(NB)]
                s_cs = [k.dsem("s_cs", _i) for _i in range(NB)]
                s_qk = [k.dsem("s_qk", _i) for _i in range(NB)]

                groups = [(s, g) for s in range(NS) for g in range(NG)]
                pcount = [0]

                def stA0(G):
                    s, g = groups[G]
                    gb = G % NB
                    k.dma("act", s_cs[gb], cs[gb][:, 0, :], cos_d[:, g * 512:(g + 1) * 512], Wd=[b_cs[gb]])
                    k.dma("act", s_cs[gb], cs[gb][:, 1, :], sin_d[:, g * 512:(g + 1) * 512], Wd=[b_cs[gb]])
                    for jj in range(4):
                        j = 4 * g + jj
                        i = jj
                        k.dma("act", s_xt[i], xt[i][:], xsrc[s][j * 128:(j + 1) * 128, :],
                              R=([b_xsrc[s]] if b_xsrc else []), W=[b_xt[i]])
                        rms_rstd(None, xt[i][:], b_xt[i], D, st3[i][:, 0:1], st3[i][:, 1:2],
                                 st3[i][:, 2:3], junk[i][:], b_st[i])
                        k.op("dve", lambda e, i=i: e.scalar_tensor_tensor(
                            out=hb[i][:], in0=xt[i][:], scalar=st3[i][:, 2:3], in1=gA[:],
                            op0=ALU.mult, op1=ALU.mult), R=[b_xt[i], b_st[i], b_gA], W=[b_hb[i]])

                def stA1(G):
                    s, g = groups[G]
                    gb = G % NB

                    def T(jj):
                        i = jj
                        ti = jj % 2
                        k.op("pe", lambda e: [e.transpose(out=tp_ps[ti][:, c * 128:(c + 1) * 128],
                                                          in_=hb[i][:, c * 128:(c + 1) * 128],
                                                          identity=identb[:]) for c in range(8)],
                             R=[b_hb[i], b_const], W=[b_tp[ti]])
                        k.op("act", lambda e: e.copy(
                            out=hT[gb][:, :, jj * 128:(jj + 1) * 128],
                            in_=tp_ps[ti][:].rearrange("p (c t) -> p c t", c=8)), R=[b_tp[ti]],
                             W=([b_hT[gb]] if jj == 0 else []), Wd=([] if jj == 0 else [b_hT[gb]]))

                    def V(jj):
                        j = 4 * g + jj
                        i = jj % NB
                        k.op("pe", lambda e: [e.matmul(
                            v_ps[:], lhsT=hT[gb][:, c, jj * 128:(jj + 1) * 128], rhs=Wbf[:, c, 1024:1536],
                            start=(c == 0), stop=(c == 7)) for c in range(8)] + [e.matmul(
                            vb_ps[:, 0:128], lhsT=hT[gb][:, c, jj * 128:(jj + 1) * 128], rhs=Wbf[:, c, 2176:2304],
                            start=(c == 0), stop=(c == 7)) for c in range(8)],
                             R=[b_hT[gb], b_W], W=[b_vps])
                        k.op("act", lambda e: e.copy(out=vst[i][:, 0:512], in_=v_ps[:]),
                             R=[b_vps], W=[b_vst[i]])
                        k.op("act", lambda e: e.copy(out=vst[i][:, 512:640], in_=vb_ps[:, 0:128]),
                             R=[b_vps], Wd=[b_vst[i]])
                        k.dma("sp", s_vst[i], Vd[s, j * 128:(j + 1) * 128, :], vst[i][:],
                              R=[b_vst[i]], Wd=[b_Vd[s]])
                    T(0)
                    T(1)
                    V(0)
                    T(2)
                    V(1)
                    T(3)
                    V(2)
                    V(3)

                def pairs(G):
                    s, g = groups[G]
                    gb = G % NB
                    for pr in range(13):
                        c0 = PAIRCOL[pr]
                        i = pcount[0] % NB
                        pcount[0] += 1
                        k.op("pe", lambda e, i=i, c0=c0: [e.matmul(
                            x_ps[i][:], lhsT=Wbf[:, c, c0:c0 + 128], rhs=hT[gb][:, c, :],
                            start=(c == 0), stop=(c == 7)) for c in range(8)] + [e.matmul(
                            r_ps[i][:], lhsT=Wrot[:, c, c0:c0 + 128], rhs=hT[gb][:, c, :],
                            start=(c == 0), stop=(c == 7)) for c in range(8)],
                             R=[b_hT[gb], b_W, b_Wrot], W=[b_xps[i]])
                        k.op("dve", lambda e, i=i: e.tensor_tensor(
                            out=t12[i][:, 0, :], in0=x_ps[i][:], in1=cs[gb][:, 0, :], op=ALU.mult),
                             R=[b_xps[i], b_cs[gb]], W=[b_t12[i]])
                        k.op("dve", lambda e, i=i: e.tensor_tensor(
                            out=t12[i][:, 1, :], in0=r_ps[i][:], in1=cs[gb][:, 1, :], op=ALU.mult),
                             R=[b_xps[i], b_cs[gb]], Wd=[b_t12[i]])
                        k.op("pool", lambda e, i=i: e.tensor_tensor(
                            out=qk[i][:], in0=t12[i][:, 0, :], in1=t12[i][:, 1, :], op=ALU.add),
                             R=[b_t12[i]], W=[b_qk[i]])
                        k.dma("sp", s_qk[i], QT[s, pr, :, g * 512:(g + 1) * 512], qk[i][:],
                              R=[b_qk[i]], Wd=[b_QT[s]])

                stA0(0)
                for G in range(len(groups)):
                    stA1(G)
                    pairs(G)
                    if G + 1 < len(groups):
                        stA0(G + 1)
                k.barrier()

            with ExitStack() as ph:
                NCH = S // 128
                vstage = sbt(ph, "vstage", [128, NCH, 256], BF)
                Vaug = [sbt(ph, "Vaug%d" % p, [128, NCH, 4, 65], BF) for p in range(3)]
                Vbaug = sbt(ph, "Vbaug", [128, NCH, 2, 65], BF)
                QTt = [sbt(ph, "QTt%d" % i, [64, S], BF) for i in range(2)]
                KTt = [sbt(ph, "KTt%d" % i, [64, S], BF) for i in range(2)]
                acc = [sbt(ph, "acc%d" % i, [65, S], F32) for i in range(2)]
                KBt = [sbt(ph, "KBt%d" % i, [64, S], BF) for i in range(2)]
                onrm = [sbt(ph, "onrm%d" % i, [64, S], BF) for i in range(2)]
                rdb = sbt(ph, "rdb", [65, S], BF)
                b_rdb = Buf("rdb")
                pT = [sbt(ph, "pT%d" % i, [128, 384], BF) for i in range(6)]
                esink = sbt(ph, "esink", [128, 8], F32)
                s_ps = [pst(ph, "s_ps%d" % i, [128, 512], F32) for i in range(3)]
                o_ps = [pst(ph, "o_ps%d" % i, [65, 512], F32) for i in range(2)]
                bc_ps = [pst(ph, "bc_ps%d" % i, [64, 512], F32) for i in range(2)]
                b_vstage = Buf("vstage")
                b_Vaug = [Buf("Vaug") for _ in range(3)]
                b_Vbaug = Buf("Vbaug")
                b_QTt = [Buf("QTt") for _ in range(2)]
                b_KTt = [Buf("KTt") for _ in range(2)]
                b_acc = [Buf("acc") for _ in range(2)]
                b_KBt = [Buf("KBt") for _ in range(2)]
                b_rden = Buf("rden")
                b_onrm = [Buf("onrm") for _ in range(2)]
                b_pT = [Buf("pT") for _ in range(6)]
                b_esink = Buf("esink")
                b_sps = [Buf("sps") for _ in range(3)]
                b_ops = [Buf("ops") for _ in range(2)]
                b_bc = [Buf("bc") for _ in range(2)]
                s_vstage = k.dsem("s_vstage")
                s_QTt = [k.dsem("s_QTt", _i) for _i in range(2)]
                s_KTt = [k.dsem("s_KTt", _i) for _i in range(2)]
                s_KBt = [k.dsem("s_KBt", _i) for _i in range(2)]
                s_onrm = [k.dsem("s_onrm", _i) for _i in range(2)]
                s_es = k.dsem("s_es")
                cnt = dict(u=0, o=0, bc=0)

                k.dma("sp", s_es, esink[:], sink[l:l + 1, :].partition_broadcast(128), W=[b_esink])
                k.op("act", lambda e: e.activation(out=esink[:], in_=esink[:], func=AF.Exp), R=[b_esink], W=[b_esink])
                for p in range(3):
                    k.op("pool", lambda e, p=p: e.memset(Vaug[p][:], 1.0), W=[b_Vaug[p]])
                k.op("pool", lambda e: e.memset(Vbaug[:], 1.0), W=[b_Vbaug])

                DP = 5
                NSP = 3

                def band_units(units, qv, b_q, kv_, b_k, vfn, b_v, n, w, mask, evac):
                    nblk = (n + 511) // 512
                    for b in range(nblk):
                        q0 = 512 * b
                        q1 = min(n, q0 + 512)
                        blk = dict(oi=None)
                        first = True
                        ulist = []
                        for m in range((n + 127) // 128):
                            base = 128 * m - w
                            lo = max(q0, base, 0)
                            hi = min(q1, 128 * m + 128 + w, n)
                            if hi <= lo:
                                continue
                            ulist.append((m, base, lo, hi, first))
                            first = False
                        for ix, (m, base, lo, hi, first) in enumerate(ulist):
                            N = hi - lo
                            kn = min(128, n - 128 * m)

                            def fS(si, m=m, lo=lo, hi=hi, N=N, kn=kn, base=base):
                                k.op("pe", lambda e: e.matmul(
                                    s_ps[si][0:kn, 0:N], lhsT=kv_(128 * m, 128 * m + kn), rhs=qv(lo, hi),
                                    start=True, stop=True), R=[b_q, b_k], W=[b_sps[si]])

                            def fE(si, pi_, N=N, kn=kn, lo=lo, hi=hi, base=base):
                                k.op("act", lambda e: e.activation(
                                    out=pT[pi_][0:kn, 0:N], in_=s_ps[si][0:kn, 0:N], func=AF.Exp, scale=0.125),
                                     R=[b_sps[si]], W=[b_pT[pi_]])
                                meng = "dve"
                                k.op(meng, lambda e: e.tensor_tensor(
                                    out=pT[pi_][0:kn, 0:N], in0=pT[pi_][0:kn, 0:N],
                                    in1=mask[0:kn, lo - base:hi - base], op=ALU.mult),
                                     R=[b_pT[pi_], b_m01], W=[b_pT[pi_]])

                            def fPV(pi_, m=m, lo=lo, hi=hi, N=N, kn=kn, q0=q0, first=first, blk=blk):
                                if first:
                                    blk["oi"] = cnt["o"] % 2
                                    cnt["o"] += 1
                                oi = blk["oi"]
                                k.op("pe", lambda e: e.matmul(
                                    o_ps[oi][0:65, lo - q0:hi - q0], lhsT=vfn(m)[0:kn, :], rhs=pT[pi_][0:kn, 0:N],
                                    start=first, stop=False, skip_group_check=True),
                                     R=[b_pT[pi_], b_v], W=[b_ops[oi]])

                            post = []
                            if ix == len(ulist) - 1:
                                post.append(lambda b=b, q0=q0, q1=q1, blk=blk: evac(
                                    b, q0, q1, o_ps[blk["oi"]], b_ops[blk["oi"]]))
                            units.append(dict(pre=[], S=fS, E=fE, PV=fPV, post=post))

                deferred = []

                def after_pv():
                    for d_ in deferred:
                        d_[0] -= 1
                    while deferred and deferred[0][0] <= 0:
                        deferred.pop(0)[1]()

                def run_units(units):
                    q = []

                    def pop_pv():
                        v, vi = q.pop(0)
                        v["PV"](vi)
                        for f in v["post"]:
                            f()
                        after_pv()
                    for u in units:
                        if u.get("flush"):
                            while q:
                                pop_pv()
                        for f in u["pre"]:
                            f()
                        ui = cnt["u"]
                        cnt["u"] += 1
                        si = ui % NSP
                        pi_ = ui % (DP + 1)
                        u["S"](si)
                        u["E"](si, pi_)
                        q.append((u, pi_))
                        if len(q) > DP:
                            pop_pv()
                    while q:
                        pop_pv()
                    while deferred:
                        deferred.pop(0)[1]()

                def normalize_store(s, row0, oi, ai):
                    k.op("act", lambda e: e.activation(out=acc[ai][64:65, :], in_=acc[ai][64:65, :], func=AF.Ln),
                         R=[b_acc[ai]], W=[b_acc[ai]])
                    k.op("act", lambda e: e.activation(out=rdb[64:65, :], in_=acc[ai][64:65, :], func=AF.Exp,
                                                       scale=-1.0), R=[b_acc[ai]], W=[b_rdb])

                    def part2():
                        for g in range(NG):
                            bi = cnt["bc"] % 2
                            cnt["bc"] += 1
                            k.op("pe", lambda e, g=g, bi=bi: e.matmul(
                                bc_ps[bi][:], lhsT=onesbp[64:65, 0:64], rhs=rdb[64:65, g * 512:(g + 1) * 512],
                                start=True, stop=True), R=[b_rdb, b_const], W=[b_bc[bi]])
                            k.op("dve", lambda e, g=g, bi=bi: e.tensor_tensor(
                                out=onrm[oi][:, g * 512:(g + 1) * 512], in0=bc_ps[bi][:],
                                in1=acc[ai][0:64, g * 512:(g + 1) * 512], op=ALU.mult),
                                 R=[b_bc[bi], b_acc[ai]], Wd=[b_onrm[oi]])
                        k.dma("sp", s_onrm[oi], mixT[s, row0:row0 + 64, :], onrm[oi][:], R=[b_onrm[oi]],
                              Wd=[b_mixT[s]])
                    deferred.append([10, part2])

                hcount = 0
                for s in range(NS):
                    units = []
                    heads = []
                    for hg in range(2):
                        def load_v(s=s, hg=hg):
                            for p, d in enumerate(PATTERNS):
                                n = S // d
                                nch = n // 128
                                vsrc = Vd[s].rearrange("(m k r) c -> k r m c", k=128, r=d)
                                for r in range(d):
                                    k.dma("sp", s_vstage, vstage[:, r * nch:(r + 1) * nch, :],
                                          vsrc[:, r, :, hg * 256:(hg + 1) * 256],
                                          R=[b_Vd[s]], W=([b_vstage] if r == 0 else []), Wd=([] if r == 0 else [b_vstage]))
                                for hh in range(4):
                                    k.op("dve" if hh % 2 == 0 else "act", lambda e, p=p, hh=hh: (
                                        e.tensor_copy(out=Vaug[p][:, :, hh, 0:64], in_=vstage[:, :, hh * 64:(hh + 1) * 64])
                                        if hh % 2 == 0 else
                                        e.copy(out=Vaug[p][:, :, hh, 0:64], in_=vstage[:, :, hh * 64:(hh + 1) * 64])),
                                         R=[b_vstage], W=([b_Vaug[p]] if hh == 0 else []), Wd=([] if hh == 0 else [b_Vaug[p]]))
                        for hh in range(4):
                            h = hg * 4 + hh
                            qi = hcount % 2
                            hcount += 1

                            def load_qk(s=s, h=h, qi=qi):
                                k.dma("sp", s_QTt[qi], QTt[qi][:], QT[s, h // 2, (h % 2) * 64:(h % 2) * 64 + 64, :],
                                      R=[b_QT[s]], W=[b_QTt[qi]])
                                k.dma("sp", s_KTt[qi], KTt[qi][:], QT[s, 4 + h // 2, (h % 2) * 64:(h % 2) * 64 + 64, :],
                                      R=[b_QT[s]], W=[b_KTt[qi]])
                            u0 = len(units)
                            for p, d in enumerate(PATTERNS):
                                n = S // d
                                nch = n // 128
                                for r in range(d):
                                    def qv(lo, hi, r=r, d=d, qi=qi):
                                        return QTt[qi][:, r + d * lo:r + d * (hi - 1) + 1:d]

                                    def kv_(lo, hi, r=r, d=d, qi=qi):
                                        return KTt[qi][:, r + d * lo:r + d * (hi - 1) + 1:d]

                                    def vfn(m, p=p, r=r, nch=nch, hh=hh):
                                        return Vaug[p][:, r * nch + m, hh, :]

                                    def evac(b, q0, q1, ops, b_o, p=p, r=r, d=d, qi=qi):
                                        dst = acc[qi][:, r + d * q0:r + d * (q1 - 1) + 1:d]
                                        if p == 0:
                                            k.op("act", lambda e: e.copy(out=dst, in_=ops[0:65, 0:q1 - q0]),
                                                 R=[b_o], Wd=[b_acc[qi]])
                                        else:
                                            k.op("dve", lambda e: e.tensor_tensor(
                                                out=dst, in0=ops[0:65, 0:q1 - q0], in1=dst, op=ALU.add),
                                                 R=[b_o, b_acc[qi]], W=[b_acc[qi]])
                                    band_units(units, qv, b_QTt[qi], kv_, b_KTt[qi], vfn, b_Vaug[p], n, 64, mA01, evac)
                            if hh == 0:
                                units[u0]["pre"].append(load_v)
                                units[u0]["flush"] = True
                            heads.append((u0, load_qk))
                            units[-1]["post"].append(lambda s=s, h=h, qi=qi: normalize_store(s, h * 64, qi, qi))
                    def load_vb(s=s):
                        vsrc = Vd[s].rearrange("(m k) c -> k m c", k=128)
                        k.dma("sp", s_vstage, vstage[:, :, 0:128], vsrc[:, :, 512:640], R=[b_Vd[s]], W=[b_vstage])
                        for kvh in range(2):
                            k.op("dve", lambda e, kvh=kvh: e.tensor_copy(
                                out=Vbaug[:, :, kvh, 0:64], in_=vstage[:, :, kvh * 64:(kvh + 1) * 64]),
                                 R=[b_vstage], W=([b_Vbaug] if kvh == 0 else []), Wd=([] if kvh == 0 else [b_Vbaug]))
                    for kvh in range(2):
                        ki = kvh % 2
                        for g4 in range(4):
                            h = kvh * 4 + g4
                            qi = hcount % 2
                            hcount += 1

                            def load_qk(s=s, h=h, qi=qi, kvh=kvh, ki=ki, g4=g4):
                                if g4 == 0:
                                    k.dma("sp", s_KBt[ki], KBt[ki][:], QT[s, 12, kvh * 64:kvh * 64 + 64, :],
                                          R=[b_QT[s]], W=[b_KBt[ki]])
                                k.dma("sp", s_QTt[qi], QTt[qi][:], QT[s, 8 + h // 2, (h % 2) * 64:(h % 2) * 64 + 64, :],
                                      R=[b_QT[s]], W=[b_QTt[qi]])

                            def qv(lo, hi, qi=qi):
                                return QTt[qi][:, lo:hi]

                            def kv_(lo, hi, ki=ki):
                                return KBt[ki][:, lo:hi]

                            def vfn(m, kvh=kvh):
                                return Vbaug[:, m, kvh, :]

                            def evac(b, q0, q1, ops, b_o, qi=qi):
                                k.op("act", lambda e: e.copy(out=acc[qi][:, q0:q1], in_=ops[0:65, 0:q1 - q0]),
                                     R=[b_o], Wd=[b_acc[qi]])
                            u0 = len(units)
                            band_units(units, qv, b_QTt[qi], kv_, b_KBt[ki], vfn, b_Vbaug, S, 128, mB01, evac)
                            if kvh == 0 and g4 == 0:
                                units[u0]["pre"].append(load_vb)
                                units[u0]["flush"] = True
                            heads.append((u0, load_qk))

                            def fin(s=s, h=h, qi=qi):
                                k.op("dve", lambda e: e.tensor_scalar(
                                    out=acc[qi][64:65, :], in0=acc[qi][64:65, :], scalar1=esink[64:65, h:h + 1],
                                    scalar2=None, op0=ALU.add), R=[b_acc[qi], b_esink], W=[b_acc[qi]])
                                normalize_store(s, 512 + h * 64, qi, qi)
                            units[-1]["post"].append(fin)
                    for hi_, (u0_, lq) in enumerate(heads):
                        if hi_ == 0:
                            units[u0_]["pre"].insert(0, lq)
                        else:
                            units[heads[hi_ - 1][0]]["pre"].append(lq)
                    run_units(units)
                k.barrier()

            with ExitStack() as ph:
                Wof = sbt(ph, "Wof", [128, 8, D], F32)
                Wo = sbt(ph, "Wo", [128, 8, D], BF)
                gm = sbt(ph, "gm", [128, 8], F32)
                gF = sbt(ph, "gF", [128, D], F32)
                wr = sbt(ph, "wr", [128, 8, NE], F32)
                b_Wof = Buf("Wof")
                b_Wo = Buf("Wo")
                b_gm = Buf("gm")
                b_gF = Buf("gF")
                b_wr = Buf("wr")
                wsem = k.dsem("wsemC")
                k.dma("sp", wsem, Wof[:], w_out[l].rearrange("(c p) n -> p c n", p=128), W=[b_Wof])
                for c in range(8):
                    k.dma("sp", k.dsem("gmsem"), gm[:, c:c + 1], g_mix[l, c * 128:(c + 1) * 128].rearrange("(p o) -> p o", o=1),
                          Wd=[b_gm])
                k.dma("sp", k.dsem("gFsem"), gF[:], g_ffn[l:l + 1, :].partition_broadcast(128), W=[b_gF])
                k.dma("sp", k.dsem("wrsem"), wr[:], w_router[l].rearrange("(c p) n -> p c n", p=128), W=[b_wr])
                for c in range(8):
                    k.op("dve" if c % 2 == 0 else "act", lambda e, c=c: (
                        e.tensor_scalar(out=Wo[:, c, :], in0=Wof[:, c, :], scalar1=gm[:, c:c + 1], scalar2=None,
                                        op0=ALU.mult) if c % 2 == 0 else
                        e.activation(out=Wo[:, c, :], in_=Wof[:, c, :], func=AF.Copy, scale=gm[:, c:c + 1])),
                         R=[b_Wof, b_gm], Wd=[b_Wo])

                NB = 2
                NT3 = 3
                mT = [sbt(ph, "mT%d" % i, [128, 8, 512], BF) for i in range(NB)]
                sq = [sbt(ph, "sq%d" % i, [128, 8, 512], BF) for i in range(NB)]
                onesb = sbt(ph, "onesb", [128, 2], BF)
                NBIG = 3
                NSM = 6
                xt = [sbt(ph, "xtC%d" % i, [128, D], F32) for i in range(NBIG)]
                tmp = [sbt(ph, "tmpC%d" % i, [128, D], F32) for i in range(2)]
                xn = [sbt(ph, "xnC%d" % i, [128, D], F32) for i in range(NBIG)]
                junk = [sbt(ph, "junkC%d" % i, [128, D], BF) for i in range(2)]
                h2f = [sbt(ph, "h2f%d" % i, [128, D], F32) for i in range(NBIG)]
                h2b = [sbt(ph, "h2b%d" % i, [128, D], BF) for i in range(NBIG)]
                h2T = [sbt(ph, "h2T%d" % i, [128, 8, 128], F32) for i in range(NBIG)]
                stc = [sbt(ph, "stc%d" % i, [128, 16], F32) for i in range(NSM)]
                lge = [sbt(ph, "lge%d" % i, [128, 2, NE], F32) for i in range(NSM)]
                ssq_ps = pst(ph, "ssq_ps", [128, 512], F32)
                ya_ps = pst(ph, "ya_ps", [128, 1024], F32)
                yb_ps = pst(ph, "yb_ps", [128, 512], F32)
                tpf_ps = pst(ph, "tpf_ps", [128, 8, 128], F32)
                lg_ps = pst(ph, "lg_ps", [128, 512], F32)
                at_ps = pst(ph, "at_ps", [NE, 512], F32)
                b_mT = [Buf("mT") for _ in range(NB)]
                b_sq = [Buf("sq") for _ in range(NB)]
                b_onesb = Buf("onesb")
                b_xt = [Buf("xtC") for _ in range(NBIG)]
                b_tmp = [Buf("tmp") for _ in range(2)]
                b_xn = [Buf("xn") for _ in range(NBIG)]
                b_junk = [Buf("junk") for _ in range(2)]
                b_h2f = [Buf("h2f") for _ in range(NBIG)]
                b_h2b = [Buf("h2b") for _ in range(NBIG)]
                b_h2T = [Buf("h2T") for _ in range(NBIG)]
                b_stc = [Buf("stc") for _ in range(NSM)]
                b_stn = [Buf("stn") for _ in range(NSM)]
                b_lge = [Buf("lge") for _ in range(NSM)]
                b_ssq = Buf("ssqps")
                b_ya = Buf("ya")
                b_yb = Buf("yb")
                b_tpf = Buf("tpf")
                b_lg = Buf("lg")
                b_at = Buf("at")
                s_mT = [k.dsem("s_mT", _i) for _i in range(NB)]
                s_xt = [k.dsem("s_xtC", _i) for _i in range(NBIG)]
                s_xn = [k.dsem("s_xn", _i) for _i in range(NBIG)]
                s_h2b = [k.dsem("s_h2b", _i) for _i in range(NBIG)]
                k.op("dve", lambda e: e.memset(onesb[:], 1.0), W=[b_onesb])

                groups = [(s, g) for s in range(NS) for g in range(NG)]
                tiles = [(Gi, jj) for Gi in range(len(groups)) for jj in range(4)]

                def grp_pro(Gi):
                    s, g = groups[Gi]
                    gb = Gi % NB
                    k.dma("sp", s_mT[gb], mT[gb][:],
                          mixT[s].rearrange("(c p) t -> p c t", p=128)[:, :, g * 512:(g + 1) * 512],
                          R=[b_mixT[s]], W=[b_mT[gb]])
                    k.op("act", lambda e: e.activation(out=sq[gb][:], in_=mT[gb][:], func=AF.Square),
                         R=[b_mT[gb]], W=[b_sq[gb]])

                def st0(t):
                    Gi, jj = tiles[t]
                    s, g = groups[Gi]
                    gb = Gi % NB
                    j = 4 * g + jj
                    i = t % NBIG
                    m_ = t % NSM
                    cols = slice(jj * 128, (jj + 1) * 128)
                    k.dma("act", s_xt[i], xt[i][:], xsrc[s][j * 128:(j + 1) * 128, :],
                          R=([b_xsrc[s]] if b_xsrc else []), W=[b_xt[i]])
                    k.op("pe", lambda e: [e.matmul(
                        ssq_ps[:, 0:1], lhsT=sq[gb][:, c, cols], rhs=onesb[:, 0:1],
                        start=(c == 0), stop=(c == 3)) for c in range(4)] + [e.matmul(
                        ssq_ps[:, 1:2], lhsT=sq[gb][:, c, cols], rhs=onesb[:, 0:1],
                        start=False, stop=(c == 7), skip_group_check=True) for c in range(4, 8)],
                         R=[b_sq[gb], b_onesb], W=[b_ssq])
                    k.op("act", lambda e: e.activation(
                        out=stc[m_][:, 0:2], in_=ssq_ps[:, 0:2], func=AF.Sqrt, bias=epst[:, 0:1],
                        scale=1.0 / 512), R=[b_ssq, b_const], W=[b_stc[m_]])
                    k.op("dve", lambda e: e.reciprocal(out=stc[m_][:, 2:4], in_=stc[m_][:, 0:2]),
                         R=[b_stc[m_]], W=[b_stc[m_]])

                def st1(t):
                    Gi, jj = tiles[t]
                    s, g = groups[Gi]
                    gb = Gi % NB
                    j = 4 * g + jj
                    i = t % NBIG
                    i2 = t % 2
                    m_ = t % NSM
                    cols = slice(jj * 128, (jj + 1) * 128)
                    k.op("pe", lambda e: [e.matmul(
                        ya_ps[:, half * 512:(half + 1) * 512], lhsT=mT[gb][:, c, cols],
                        rhs=Wo[:, c, half * 512:(half + 1) * 512],
                        start=(c == 0), stop=(c == 3)) for half in range(2) for c in range(4)],
                         R=[b_mT[gb], b_Wo], W=[b_ya])
                    k.op("dve", lambda e: e.scalar_tensor_tensor(
                        out=tmp[i2][:], in0=ya_ps[:], scalar=stc[m_][:, 2:3], in1=xt[i][:],
                        op0=ALU.mult, op1=ALU.add), R=[b_ya, b_stc[m_], b_xt[i]], W=[b_tmp[i2]])
                    for half in range(2):
                        hs = slice(half * 512, (half + 1) * 512)
                        k.op("pe", lambda e, hs=hs: [e.matmul(
                            yb_ps[:], lhsT=mT[gb][:, c, cols], rhs=Wo[:, c, hs],
                            start=(c == 4), stop=(c == 7)) for c in range(4, 8)],
                             R=[b_mT[gb], b_Wo], W=[b_yb])
                        k.op("dve", lambda e, hs=hs: e.scalar_tensor_tensor(
                            out=xn[i][:, hs], in0=yb_ps[:], scalar=stc[m_][:, 3:4], in1=tmp[i2][:, hs],
                            op0=ALU.mult, op1=ALU.add), R=[b_yb, b_stc[m_], b_tmp[i2]],
                             W=([b_xn[i]] if half == 0 else []), Wd=([] if half == 0 else [b_xn[i]]))
                    k.dma("sp", s_xn[i], xr[s][j * 128:(j + 1) * 128, :], xn[i][:], R=[b_xn[i]], Wd=[b_xr[s]])
                    rms_rstd(None, xn[i][:], b_xn[i], D, stc[m_][:, 4:5], stc[m_][:, 5:6], stc[m_][:, 6:7],
                             junk[i2][:], b_stn[m_])
                    k.op("dve", lambda e: e.scalar_tensor_tensor(
                        out=h2f[i][:], in0=xn[i][:], scalar=stc[m_][:, 6:7], in1=gF[:],
                        op0=ALU.mult, op1=ALU.mult), R=[b_xn[i], b_stn[m_], b_gF], W=[b_h2f[i]])
                    k.op("pool", lambda e: e.tensor_copy(out=h2b[i][:], in_=h2f[i][:]),
                         R=[b_h2f[i]], W=[b_h2b[i]])
                    k.dma("sp", s_h2b[i], h2d[s][j * 128:(j + 1) * 128, :], h2b[i][:],
                          R=[b_h2b[i]], Wd=[b_h2d[s]])

                def st2a(t):
                    i = t % NBIG
                    k.op("pe", lambda e: [e.transpose(out=tpf_ps[:, c, :], in_=h2f[i][:, c * 128:(c + 1) * 128],
                                                      identity=identf[:]) for c in range(8)],
                         R=[b_h2f[i], b_const], W=[b_tpf])
                    k.op("act", lambda e: e.copy(out=h2T[i][:], in_=tpf_ps[:]), R=[b_tpf], W=[b_h2T[i]])

                def st2b(t):
                    i = t % NBIG
                    m_ = t % NSM
                    k.op("pe", lambda e: [e.matmul(lg_ps[:, 0:NE], lhsT=h2T[i][:, c, :], rhs=wr[:, c, :],
                                                   start=(c == 0), stop=(c == 7)) for c in range(8)],
                         R=[b_h2T[i], b_wr], W=[b_lg])
                    k.op("dve", lambda e: e.tensor_reduce(out=stc[m_][:, 8:9], in_=lg_ps[:, 0:NE], axis=AX.X,
                                                          op=ALU.max, negate=True),
                         R=[b_lg], W=[b_lge[m_]])
                    k.op("act", lambda e: e.activation(out=lge[m_][:, 0, :], in_=lg_ps[:, 0:NE], func=AF.Exp,
                                                       bias=stc[m_][:, 8:9], accum_out=stc[m_][:, 9:10]),
                         R=[b_lg, b_lge[m_]], W=[b_lge[m_]])
                    k.op("dve", lambda e: e.reciprocal(out=stc[m_][:, 10:11], in_=stc[m_][:, 9:10]),
                         R=[b_lge[m_]], W=[b_lge[m_]])
                    k.op("dve", lambda e: e.tensor_scalar(
                        out=lge[m_][:, 1, :], in0=lge[m_][:, 0, :], scalar1=stc[m_][:, 10:11], scalar2=None,
                        op0=ALU.mult), R=[b_lge[m_]], W=[b_lge[m_]])

                def st2c(t):
                    Gi, jj = tiles[t]
                    s, g = groups[Gi]
                    m_ = t % NSM
                    cols = slice(jj * 128, (jj + 1) * 128)
                    k.op("pe", lambda e: e.transpose(
                        out=at_ps[:, cols], in_=lge[m_][:, 1, :], identity=identf[:]),
                         R=[b_lge[m_], b_const], Wd=[b_at])
                    if jj == 3:
                        k.op("act", lambda e: e.copy(
                            out=affT[s * 32:s * 32 + NE, g * 512:(g + 1) * 512], in_=at_ps[:]),
                             R=[b_at], Wd=[b_affT])

                grp_pro(0)
                NTl = len(tiles)
                stages = [st0, st1, st2a, st2b, st2c]
                for step in range(NTl + len(stages) - 1):
                    for si_, stf_ in enumerate(stages):
                        t = step - si_
                        if 0 <= t < NTl:
                            stf_(t)
                    if step < NTl and tiles[step][1] == 1 and tiles[step][0] + 1 < len(groups):
                        grp_pro(tiles[step][0] + 1)
                k.barrier()

            with ExitStack() as ph:
                NP = 32 * (NS - 1) + NE
                work = sbt(ph, "work", [NP, S], F32)
                vals = sbt(ph, "vals", [NP, C], F32)
                idxu = sbt(ph, "idxu", [NP, C], U32)
                idxf = sbt(ph, "idxf", [NP, C], F32)
                tpi_ps = pst(ph, "tpi_ps", [128, 512], F32)[:, 0:NCC * 48].rearrange("p (c n) -> p c n", n=48)
                tpv_ps = pst(ph, "tpv_ps", [128, 512], F32)[:, 0:NCC * 48].rearrange("p (c n) -> p c n", n=48)
                b_work = Buf("work")
                b_vals = Buf("vals")
                b_idxu = Buf("idxu")
                b_idxf = Buf("idxf")
                b_tpi = Buf("tpi")
                k.op("dve", lambda e: e.tensor_copy(out=work[:], in_=affT[0:NP, :]), R=[b_affT], W=[b_work])
                for it in range(C // 8):
                    sl = slice(it * 8, it * 8 + 8)
                    k.op("dve", lambda e, sl=sl: e.max(out=vals[:, sl], in_=work[:]), R=[b_work], Wd=[b_vals])
                    k.op("dve", lambda e, sl=sl: e.max_index(out=idxu[:, sl], in_max=vals[:, sl], in_values=work[:]),
                         R=[b_work, b_vals], Wd=[b_idxu])
                    k.op("dve", lambda e, sl=sl: e.match_replace(out=work[:], in_to_replace=vals[:, sl],
                                                                 in_values=work[:], imm_value=-1.0),
                         R=[b_vals, b_idxu], W=[b_work])
                k.op("dve", lambda e: e.tensor_copy(out=idxf[:], in_=idxu[:]), R=[b_idxu], W=[b_idxf])
                k.op("pe", lambda e: [e.transpose(out=tpi_ps[:, cc, 0:NP], in_=idxf[:, cc * 128:(cc + 1) * 128],
                                                  identity=identf[0:NP, 0:NP]) for cc in range(NCC)] +
                     [e.transpose(out=tpv_ps[:, cc, 0:NP], in_=vals[:, cc * 128:(cc + 1) * 128],
                                  identity=identf[0:NP, 0:NP]) for cc in range(NCC)],
                     R=[b_idxf, b_vals, b_const], W=[b_tpi])
                k.op("dve", lambda e: e.tensor_copy(out=idxT[:, :, 0:NP], in_=tpi_ps[:, :, 0:NP]), R=[b_tpi], W=[b_idxT])
                k.op("act", lambda e: e.copy(out=gateT[:, :, 0:NP], in_=tpv_ps[:, :, 0:NP]), R=[b_tpi], W=[b_gateT])
                if dbg and l == 0:
                    dsm = k.dsem("dbgsem")
                    k.dma("sp", dsm, dbg_idx, idxT[:], R=[b_idxT])
                    k.dma("sp", dsm, dbg_gate, gateT[:], R=[b_gateT])
                    k.dma("sp", dsm, dbg_aff, affT[:], R=[b_affT])
                k.barrier()

            with ExitStack() as ph:
              if "E" not in skip:
                  NWB = 2
                  Wg = [sbt(ph, "Wg%d" % i, [128, 8, D], BF) for i in range(NWB)]
                  Wu = [sbt(ph, "Wu%d" % i, [128, 8, D], BF) for i in range(NWB)]
                  Wd_ = [sbt(ph, "Wd%d" % i, [128, 8, D], BF) for i in range(NWB)]
                  NXE = 4
                  NYE = 4
                  xe = [sbt(ph, "xe%d" % i, [128, NCC, D], BF) for i in range(NXE)]
                  xeT = [sbt(ph, "xeT%d" % i, [128, 8, C], BF) for i in range(2)]
                  sg = [sbt(ph, "sg%d" % i, [128, C], F32) for i in range(2)]
                  hidT = [sbt(ph, "hidT%d" % i, [128, 8, C], BF) for i in range(2)]
                  ye = [sbt(ph, "ye%d" % i, [128, D], F32) for i in range(NYE)]
                  tpe_ps = [pst(ph, "tpe_ps%d" % i, [128, 1024], BF) for i in range(2)]
                  g_ps = [pst(ph, "g_ps%d" % i, [128, 512], F32) for i in range(2)]
                  u_ps = [pst(ph, "u_ps%d" % i, [128, 512], F32) for i in range(2)]
                  y_ps = [pst(ph, "y_ps%d" % i, [128, 512], F32) for i in range(2)]
                  b_Wg = [Buf("Wg") for _ in range(NWB)]
                  b_xe = [Buf("xe") for _ in range(NXE)]
                  b_xeT = [Buf("xeT") for _ in range(2)]
                  b_sg = [Buf("sg") for _ in range(2)]
                  b_hidT = [Buf("hidT") for _ in range(2)]
                  b_ye = [Buf("ye") for _ in range(NYE)]
                  b_tpe = [Buf("tpe") for _ in range(2)]
                  b_gu = [Buf("gu") for _ in range(2)]
                  b_yps = [Buf("yps") for _ in range(2)]
                  s_W = [k.dsem("s_W", _i) for _i in range(NWB)]
                  s_xe = [k.dsem("s_xe", _i) for _i in range(NXE)]
                  s_ye = [k.dsem("s_ye", _i) for _i in range(NYE)]
                  cnt = dict(xe=0, tp=0, gu=0, y=0, ye=0)

                  def load_w(e_):
                      wi = e_ % NWB
                      for (wt, src) in ((Wg[wi], w_gate), (Wu[wi], w_up), (Wd_[wi], w_down)):
                          k.dma("pool", s_W[wi], wt[:], src[l, e_].rearrange("(c p) n -> p c n", p=128),
                                W=([b_Wg[wi]] if wt is Wg[wi] else []), Wd=([] if wt is Wg[wi] else [b_Wg[wi]]))

                  bc_reg = nc.gpsimd.to_reg(S - 1)
                  items = [(e_, s) for e_ in range(NE) for s in range(NS)]

                  def gather(ii):
                      e_, s = items[ii]
                      col = s * 32 + e_
                      xi = ii % NXE
                      for cc in range(NCC):
                          k.idma(s_xe[xi], out=xe[xi][:, cc, :], out_offset=None, in_=h2d[s],
                                 in_offset=bass.IndirectOffsetOnAxis(ap=idxT[:, cc, col:col + 1], axis=0),
                                 R=[b_h2d[s], b_idxT], W=([b_xe[xi]] if cc == 0 else []),
                                 Wd=([] if cc == 0 else [b_xe[xi]]))

                  def compute(ii):
                      e_, s = items[ii]
                      wi = e_ % NWB
                      col = s * 32 + e_
                      xi = ii % NXE
                      hi_ = ii % 2
                      for c in range(8):
                          ti = cnt["tp"] % 2
                          cnt["tp"] += 1
                          k.op("pe", lambda e, c=c, ti=ti: [e.transpose(
                              out=tpe_ps[ti][:, cc * 128:(cc + 1) * 128], in_=xe[xi][:, cc, c * 128:(c + 1) * 128],
                              identity=identb[:]) for cc in range(NCC)], R=[b_xe[xi], b_const], W=[b_tpe[ti]])
                          k.op("act" if c % 2 == 0 else "dve", lambda e, c=c, ti=ti: (
                              e.copy(out=xeT[hi_][:, c, :], in_=tpe_ps[ti][:, 0:C]) if c % 2 == 0 else
                              e.tensor_copy(out=xeT[hi_][:, c, :], in_=tpe_ps[ti][:, 0:C])),
                               R=[b_tpe[ti]], W=([b_xeT[hi_]] if c == 0 else []), Wd=([] if c == 0 else [b_xeT[hi_]]))
                      for fc in range(8):
                          gi = cnt["gu"] % 2
                          cnt["gu"] += 1
                          fs = slice(fc * 128, (fc + 1) * 128)
                          k.op("pe", lambda e, fs=fs, gi=gi: [e.matmul(
                              g_ps[gi][:, 0:C], lhsT=Wg[wi][:, c, fs], rhs=xeT[hi_][:, c, :], start=(c == 0), stop=(c == 7))
                              for c in range(8)] + [e.matmul(
                              u_ps[gi][:, 0:C], lhsT=Wu[wi][:, c, fs], rhs=xeT[hi_][:, c, :], start=(c == 0), stop=(c == 7))
                              for c in range(8)], R=[b_xeT[hi_], b_Wg[wi]], W=[b_gu[gi]])
                          k.op("act", lambda e, gi=gi: e.activation(out=sg[gi][:], in_=g_ps[gi][:, 0:C], func=AF.Silu),
                               R=[b_gu[gi]], W=[b_sg[gi]])
                          k.op("dve", lambda e, gi=gi, fc=fc: e.tensor_tensor(
                              out=hidT[hi_][:, fc, :], in0=u_ps[gi][:, 0:C], in1=sg[gi][:], op=ALU.mult),
                               R=[b_gu[gi], b_sg[gi]], W=([b_hidT[hi_]] if fc == 0 else []),
                               Wd=([] if fc == 0 else [b_hidT[hi_]]))
                      for cc in range(NCC):
                          yi = cnt["ye"] % NYE
                          cnt["ye"] += 1
                          for half in range(2):
                              pi_ = cnt["y"] % 2
                              cnt["y"] += 1
                              hs = slice(half * 512, (half + 1) * 512)
                              k.op("pe", lambda e, cc=cc, hs=hs, pi_=pi_: [e.matmul(
                                  y_ps[pi_][:], lhsT=hidT[hi_][:, fc, cc * 128:(cc + 1) * 128], rhs=Wd_[wi][:, fc, hs],
                                  start=(fc == 0), stop=(fc == 7)) for fc in range(8)],
                                   R=[b_hidT[hi_], b_Wg[wi]], W=[b_yps[pi_]])
                              k.op("act", lambda e, cc=cc, hs=hs, pi_=pi_, yi=yi: e.activation(
                                  out=ye[yi][:, hs], in_=y_ps[pi_][:], func=AF.Copy,
                                  scale=gateT[:, cc, col:col + 1]), R=[b_yps[pi_], b_gateT],
                                   W=([b_ye[yi]] if half == 0 else []), Wd=([] if half == 0 else [b_ye[yi]]))
                          k.idma(s_ye[yi], out=xr[s], out_offset=bass.IndirectOffsetOnAxis(
                              ap=idxT[:, cc, col:col + 1], axis=0), in_=ye[yi][:], in_offset=None,
                                 compute_op=ALU.add, bounds_check=bc_reg, oob_is_err=True,
                                 R=[b_ye[yi], b_idxT], W=([b_xr[s]] if cc == 0 else []),
                                 Wd=([] if cc == 0 else [b_xr[s]]))

                  load_w(0)
                  for ii in range(min(2, len(items))):
                      gather(ii)
                  for ii, (e_, s) in enumerate(items):
                      if ii + 2 < len(items):
                          gather(ii + 2)
                      if s == 0 and e_ + 1 < NE:
                          load_w(e_ + 1)
                      compute(ii)
                  k.barrier()

        with ExitStack() as ph:
            gL = sbt(ph, "gL", [128, D], F32)
            b_gL = Buf("gL")
            fsem = k.dsem("fsem")
            k.dma("sp", fsem, gL[:], g_final[0:1, :].partition_broadcast(128), W=[b_gL])
            NB = 4
            xt = [sbt(ph, "xtF%d" % i, [128, D], F32) for i in range(NB)]
            yo = [sbt(ph, "yoF%d" % i, [128, D], F32) for i in range(NB)]
            junk = [sbt(ph, "junkF%d" % i, [128, D], BF) for i in range(NB)]
            stf = [sbt(ph, "stf%d" % i, [128, 4], F32) for i in range(NB)]
            b_xt = [Buf("xtF") for _ in range(NB)]
            b_yo = [Buf("yoF") for _ in range(NB)]
            b_st = [Buf("stF") for _ in range(NB)]
            s_xt = [k.dsem("s_xtF", _i) for _i in range(NB)]
            s_yo = [k.dsem("s_yoF", _i) for _i in range(NB)]
            ti = 0
            for s in range(NS):
                for j in range(NT):
                    i = ti % NB
                    ti += 1
                    k.dma("act", s_xt[i], xt[i][:], xr[s][j * 128:(j + 1) * 128, :], R=[b_xr[s]], W=[b_xt[i]])
                    rms_rstd(None, xt[i][:], b_xt[i], D, stf[i][:, 0:1], stf[i][:, 1:2], stf[i][:, 2:3],
                             junk[i][:], b_st[i])
                    k.op("dve", lambda e, i=i: e.scalar_tensor_tensor(
                        out=yo[i][:], in0=xt[i][:], scalar=stf[i][:, 2:3], in1=gL[:], op0=ALU.mult, op1=ALU.mult),
                         R=[b_xt[i], b_st[i], b_gL], W=[b_yo[i]])
                    k.dma("sp", s_yo[i], y_out[s, j * 128:(j + 1) * 128, :], yo[i][:], R=[b_yo[i]], Wd=[b_y])
            k.barrier()
    return nc


def rope_tables_T(S):
    inv = (1.0 / (np.float32(10000.0) ** (np.arange(0, HD, 2, dtype=np.float32) / np.float32(HD)))).astype(np.float32)
    ang = (np.arange(S, dtype=np.float32)[None, :] * inv[:, None]).astype(np.float32)
    cosT = np.tile(np.cos(ang).astype(np.float32), (4, 1))
    sinT = np.tile(np.sin(ang).astype(np.float32), (4, 1))
    return np.ascontiguousarray(cosT), np.ascontiguousarray(sinT)


def band_mask(w):
    kap = np.arange(128)[:, None]
    th = np.arange(128 + 2 * w)[None, :]
    valid = (kap <= th) & (th <= kap + 2 * w)
    return np.where(valid, 0.0, NEG).astype(ml_dtypes.bfloat16)


def const_inputs(S):
    cosT, sinT = rope_tables_T(S)
    return dict(cosT=cosT, sinT=sinT,
                ident_bf=np.eye(128, dtype=np.float32).astype(ml_dtypes.bfloat16),
                ident_f=np.eye(128, dtype=np.float32),
                maskA=band_mask(64), maskB=band_mask(128))


def make_in_maps(inputs, n_cores, NS):
    f = lambda a: np.ascontiguousarray(np.asarray(a, dtype=np.float32))
    x = f(inputs["x"])
    S = x.shape[1]
    L = inputs["w_in"].shape[0]
    shared = dict(
        w_in=f(inputs["w_in"]), w_out=f(inputs["w_out"]), g_attn=f(inputs["g_attn"]),
        g_mix=np.ascontiguousarray(np.concatenate([f(inputs["g_mix_a"]), f(inputs["g_mix_b"])], axis=1)),
        sink=f(inputs["sink"]).reshape(L, 8), g_ffn=f(inputs["g_ffn"]), w_router=f(inputs["w_router"]),
        w_gate=f(inputs["w_gate"]), w_up=f(inputs["w_up"]), w_down=f(inputs["w_down"]),
        g_final=f(inputs["g_final"]).reshape(1, D))
    shared.update(const_inputs(S))
    maps = []
    for c in range(n_cores):
        m = dict(shared)
        m["x"] = np.ascontiguousarray(x[c * NS:(c + 1) * NS])
        maps.append(m)
    return maps


def kernel(**inputs):
    x = np.asarray(inputs["x"])
    B, S, _ = x.shape
    L = np.asarray(inputs["w_in"]).shape[0]
    n_cores = 8
    NS = B // n_cores
    nc = build_program(S, NS, L)
    maps = make_in_maps(inputs, n_cores, NS)
    res = run_bass_kernel_spmd(nc, maps, core_ids=list(range(n_cores)))
    out = np.concatenate([np.asarray(r["y"]) for r in res.results], axis=0)
    return out.astype(np.float32)
```

```python
import numpy as np
import ml_dtypes
from contextlib import ExitStack
import concourse.bass as bass
import concourse.mybir as mybir
from concourse.bass_utils import run_bass_kernel_spmd

F32 = mybir.dt.float32
BF = mybir.dt.bfloat16
U32 = mybir.dt.uint32
I32 = mybir.dt.int32
AF = mybir.ActivationFunctionType
ALU = mybir.AluOpType
AX = mybir.AxisListType

D = 1024
HD = 64
INW = 2304
NE = 16
EPS = 1e-6
NEG = -30000.0
PATTERNS = (1, 4, 16)
PAIRCOL = [0, 128, 256, 384, 512, 640, 768, 896, 1536, 1664, 1792, 1920, 2048]


class Buf:
    __slots__ = ("w", "r", "name")

    def __init__(self, name):
        self.name = name
        self.w = {}
        self.r = {}


class KB:
    ROLL = 16000

    def __init__(self, nc, es):
        self.nc = nc
        self.es = es
        self.E = dict(pe=nc.tensor, act=nc.scalar, dve=nc.vector, pool=nc.gpsimd, sp=nc.sync)
        self.nsem = 0
        self.esem = {e: self.newsem("e_" + e) for e in self.E}
        self.waited = {e: {} for e in self.E}
        self.dsems = []
        self.dcache = {}
        self.esems_all = list(self.esem.values())

    def newsem(self, name):
        self.nsem += 1
        h = self.es.enter_context(self.nc.semaphore("%s_%d" % (name, self.nsem)))
        return [h, 0]

    def dsem(self, name, idx=0):
        key = (name, idx)
        if key not in self.dcache:
            s = self.newsem(name)
            self.dsems.append(s)
            self.dcache[key] = s
        return self.dcache[key]

    def _wait(self, eng, toks):
        need = {}
        for s, v in toks:
            k = id(s)
            if k not in need or need[k][1] < v:
                need[k] = (s, v)
        wd = self.waited[eng]
        for k, (s, v) in need.items():
            if wd.get(k, 0) < v:
                self.E[eng].wait_ge(s[0], v)
                wd[k] = v

    def pre(self, eng, R, W, Wd):
        toks = []
        for b in R:
            toks.extend(b.w.values())
        for b in W:
            toks.extend(b.w.values())
            toks.extend(b.r.values())
        for b in Wd:
            toks.extend(b.r.values())
        self._wait(eng, toks)

    def post(self, tok, R, W, Wd):
        s, v = tok
        k = id(s)
        for b in R:
            b.r[k] = tok
        for b in W:
            b.w[k] = tok
        for b in Wd:
            b.w[k] = tok

    def op(self, eng, fn, R=(), W=(), Wd=()):
        self.pre(eng, R, W, Wd)
        ins = fn(self.E[eng])
        if isinstance(ins, (list, tuple)):
            ins = ins[-1]
        s = self.esem[eng]
        if s[1] >= self.ROLL:
            s = self.esem[eng] = self.newsem("e_" + eng)
            self.esems_all.append(s)
        s[1] += 1
        ins.then_inc(s[0], 1)
        self.post((s, s[1]), R, W, Wd)

    def dma(self, q, sem, out, in_, R=(), W=(), Wd=()):
        self.pre(q, R, W, Wd)
        ins = self.E[q].dma_start(out=out, in_=in_)
        sem[1] += 16
        ins.then_inc(sem[0], 16)
        self.post((sem, sem[1]), R, W, Wd)

    def idma(self, sem, R=(), W=(), Wd=(), **kw):
        self.pre("pool", R, W, Wd)
        ins = self.nc.gpsimd.indirect_dma_start(**kw)
        sem[1] += 16
        ins.then_inc(sem[0], 16)
        self.post((sem, sem[1]), R, W, Wd)

    def barrier(self):
        toks = [(s, s[1]) for s in self.esems_all if s[1] > 0]
        toks += [(s, s[1]) for s in self.dsems if s[1] > 0]
        for e in self.E:
            self._wait(e, toks)


def build_program(S, NS, L, dbg=False, skip=()):
    C = 2 * S // NE
    NT = S // 128
    NG = S // 512
    NCC = C // 128
    nc = bass.Bass("TRN2", target_bir_lowering=False)

    def din(name, shape, dt):
        return nc.dram_tensor(name, list(shape), dt, kind="ExternalInput").ap()

    x_in = din("x", [NS, S, D], F32)
    w_in = din("w_in", [L, D, INW], F32)
    w_out = din("w_out", [L, D, D], F32)
    g_attn = din("g_attn", [L, D], F32)
    g_mix = din("g_mix", [L, D], F32)
    sink = din("sink", [L, 8], F32)
    g_ffn = din("g_ffn", [L, D], F32)
    w_router = din("w_router", [L, D, NE], F32)
    w_gate = din("w_gate", [L, NE, D, D], F32)
    w_up = din("w_up", [L, NE, D, D], F32)
    w_down = din("w_down", [L, NE, D, D], F32)
    g_final = din("g_final", [1, D], F32)
    cos_d = din("cosT", [128, S], F32)
    sin_d = din("sinT", [128, S], F32)
    identb_d = din("ident_bf", [128, 128], BF)
    identf_d = din("ident_f", [128, 128], F32)
    maskA_d = din("maskA", [128, 256], BF)
    maskB_d = din("maskB", [128, 384], BF)
    y_out = nc.dram_tensor("y", [NS, S, D], F32, kind="ExternalOutput").ap()

    skind = "ExternalOutput" if dbg else "Internal"

    def dscr(name, shape, dt):
        return nc.dram_tensor(name, list(shape), dt, kind=skind).ap()

    xr = [dscr("xr%d" % i, [S, D], F32) for i in range(NS)]
    QT = dscr("QT", [NS, 13, 128, S], BF)
    Vd = dscr("Vd", [NS, S, 640], BF)
    mixT = dscr("mixT", [NS, D, S], BF)
    h2d = [dscr("h2d%d" % i, [S, D], BF) for i in range(NS)]
    if dbg:
        dbg_idx = dscr("dbg_idx", [128, NCC, 48], I32)
        dbg_gate = dscr("dbg_gate", [128, NCC, 48], F32)
        dbg_aff = dscr("dbg_aff", [128, S], F32)

    with ExitStack() as es:
        k = KB(nc, es)

        uid = [0]

        def sbt(st, name, shape, dt):
            uid[0] += 1
            return st.enter_context(nc.sbuf_tensor("%s_%d" % (name, uid[0]), list(shape), dt))

        def pst(st, name, shape, dt):
            uid[0] += 1
            return st.enter_context(nc.psum_tensor("%s_%d" % (name, uid[0]), list(shape), dt))

        b_xr = [Buf("xr%d" % s) for s in range(NS)]
        b_QT = [Buf("QT%d" % s) for s in range(NS)]
        b_Vd = [Buf("Vd%d" % s) for s in range(NS)]
        b_mixT = [Buf("mixT%d" % s) for s in range(NS)]
        b_h2d = [Buf("h2d%d" % s) for s in range(NS)]
        b_y = Buf("y")

        identb = sbt(es, "identb", [128, 128], BF)
        identf = sbt(es, "identf", [128, 128], F32)
        maskA = sbt(es, "maskA_s", [128, 256], BF)
        maskB = sbt(es, "maskB_s", [128, 384], BF)
        onesf = sbt(es, "onesf", [128, 64], F32)
        epst = sbt(es, "epst", [128, 1], F32)
        affT = sbt(es, "affT", [128, S], F32)
        idxT = sbt(es, "idxT", [128, NCC, 48], I32)
        gateT = sbt(es, "gateT", [128, NCC, 48], F32)
        b_const = Buf("const")
        b_affT = Buf("affT")
        b_idxT = Buf("idxT")
        b_gateT = Buf("gateT")
        csem = k.dsem("csem")
        k.dma("sp", csem, identb[:], identb_d, W=[b_const])
        k.dma("sp", csem, identf[:], identf_d, Wd=[b_const])
        k.dma("sp", csem, maskA[:], maskA_d, Wd=[b_const])
        k.dma("sp", csem, maskB[:], maskB_d, Wd=[b_const])
        k.op("dve", lambda e: e.memset(onesf[:], 1.0), Wd=[b_const])
        onesbp = sbt(es, "onesbp", [128, 64], BF)
        k.op("dve", lambda e: e.memset(onesbp[:], 1.0), Wd=[b_const])
        k.op("dve", lambda e: e.memset(epst[:], EPS), Wd=[b_const])
        k.op("dve", lambda e: e.memset(affT[:], 0.0), W=[b_affT])
        mA01 = sbt(es, "mA01", [128, 256], BF)
        mB01 = sbt(es, "mB01", [128, 384], BF)
        b_m01 = Buf("m01")
        k.op("dve", lambda e: e.tensor_scalar(out=mA01[:], in0=maskA[:], scalar1=0.0, scalar2=None,
                                              op0=ALU.is_equal), R=[b_const], W=[b_m01])
        k.op("dve", lambda e: e.tensor_scalar(out=mB01[:], in0=maskB[:], scalar1=0.0, scalar2=None,
                                              op0=ALU.is_equal), R=[b_const], Wd=[b_m01])

        def rms_rstd(st_tiles, xt, b_xt, width, ssq, rt, rstd, junk, b_t):
            k.op("act", lambda e: e.activation(out=junk, in_=xt, func=AF.Square, accum_out=ssq),
                 R=[b_xt], W=[b_t])
            k.op("act", lambda e: e.activation(out=rt, in_=ssq, func=AF.Sqrt, bias=epst[:, 0:1],
                                               scale=1.0 / width), R=[b_t, b_const], W=[b_t])
            k.op("dve", lambda e: e.reciprocal(out=rstd, in_=rt), R=[b_t], W=[b_t])

        for l in range(L):
            xsrc, b_xsrc = ([x_in[i] for i in range(NS)], None) if l == 0 else (xr, b_xr)

            with ExitStack() as ph:
                Wbf = sbt(ph, "Wbf", [128, 8, INW], BF)
                Wrot = sbt(ph, "Wrot", [128, 8, INW], BF)
                gA = sbt(ph, "gA", [128, D], F32)
                b_W = Buf("Wbf")
                b_Wrot = Buf("Wrot")
                b_gA = Buf("gA")
                wsem = k.dsem("wsemA")
                wv = w_in[l].rearrange("(c p) n -> p c n", p=128)
                for c0 in range(0, INW, 1152):
                    k.dma("pool", wsem, Wbf[:, :, c0:c0 + 1152], wv[:, :, c0:c0 + 1152], Wd=[b_W])
                k.dma("sp", k.dsem("gAsem"), gA[:], g_attn[l:l + 1, :].partition_broadcast(128), W=[b_gA])
                for c in range(8):
                    for (c0, nh) in ((0, 16), (1536, 10)):
                        src = Wbf[:, c, c0:c0 + nh * 64].rearrange("p (h t i) -> p h t i", t=2, i=32)
                        dst = Wrot[:, c, c0:c0 + nh * 64].rearrange("p (h t i) -> p h t i", t=2, i=32)
                        k.op("act", lambda e, s_=src, d_=dst: e.mul(
                            out=d_[:, :, 0, :], in_=s_[:, :, 1, :], mul=-1.0), R=[b_W], Wd=[b_Wrot])
                        k.op("dve", lambda e, s_=src, d_=dst: e.tensor_copy(
                            out=d_[:, :, 1, :], in_=s_[:, :, 0, :]), R=[b_W], Wd=[b_Wrot])

                NB = 2
                xt = [sbt(ph, "xtA%d" % i, [128, D], F32) for i in range(4)]
                junk = [sbt(ph, "junkA%d" % i, [128, D], BF) for i in range(4)]
                hb = [sbt(ph, "hbA%d" % i, [128, D], BF) for i in range(4)]
                st3 = [sbt(ph, "stA%d" % i, [128, 4], F32) for i in range(4)]
                hT = [sbt(ph, "hT%d" % i, [128, 8, 512], BF) for i in range(NB)]
                vst = [sbt(ph, "vst%d" % i, [128, 640], BF) for i in range(NB)]
                cs = [sbt(ph, "cs%d" % i, [128, 2, 512], F32) for i in range(NB)]
                t12 = [sbt(ph, "t12_%d" % i, [128, 2, 512], F32) for i in range(NB)]
                qk = [sbt(ph, "qk%d" % i, [128, 512], BF) for i in range(NB)]
                tp_ps = [pst(ph, "tp_psA%d" % i, [128, D], BF) for i in range(2)]
                v_ps = pst(ph, "v_psA", [128, 512], F32)
                vb_ps = pst(ph, "vb_psA", [128, 512], F32)
                x_ps = [pst(ph, "x_psA%d" % i, [128, 512], F32) for i in range(NB)]
                r_ps = [pst(ph, "r_psA%d" % i, [128, 512], F32) for i in range(NB)]
                b_xt = [Buf("xt") for _ in range(4)]
                b_st = [Buf("st") for _ in range(4)]
                b_hb = [Buf("hb") for _ in range(4)]
                b_hT = [Buf("hT") for _ in range(NB)]
                b_vst = [Buf("vst") for _ in range(NB)]
                b_cs = [Buf("cs") for _ in range(NB)]
                b_t12 = [Buf("t12") for _ in range(NB)]
                b_qk = [Buf("qk") for _ in range(NB)]
                b_tp = [Buf("tp") for _ in range(2)]
                b_vps = Buf("vps")
                b_xps = [Buf("xps") for _ in range(NB)]
                s_xt = [k.dsem("s_xt", _i) for _i in range(4)]
                s_vst = [k.dsem("s_vst", _i) for _i in range(NB)]
                s_cs = [k.dsem("s_cs", _i) for _i in range(NB)]
                s_qk = [k.dsem("s_qk", _i) for _i in range(NB)]

                groups = [(s, g) for s in range(NS) for g in range(NG)]
                pcount = [0]

                def stA0(G):
                    s, g = groups[G]
                    gb = G % NB
                    k.dma("act", s_cs[gb], cs[gb][:, 0, :], cos_d[:, g * 512:(g + 1) * 512], Wd=[b_cs[gb]])
                    k.dma("act", s_cs[gb], cs[gb][:, 1, :], sin_d[:, g * 512:(g + 1) * 512], Wd=[b_cs[gb]])
                    for jj in range(4):
                        j = 4 * g + jj
                        i = jj
                        k.dma("act", s_xt[i], xt[i][:], xsrc[s][j * 128:(j + 1) * 128, :],
                              R=([b_xsrc[s]] if b_xsrc else []), W=[b_xt[i]])
                        rms_rstd(None, xt[i][:], b_xt[i], D, st3[i][:, 0:1], st3[i][:, 1:2],
                                 st3[i][:, 2:3], junk[i][:], b_st[i])
                        k.op("dve", lambda e, i=i: e.scalar_tensor_tensor(
                            out=hb[i][:], in0=xt[i][:], scalar=st3[i][:, 2:3], in1=gA[:],
                            op0=ALU.mult, op1=ALU.mult), R=[b_xt[i], b_st[i], b_gA], W=[b_hb[i]])

                def stA1(G):
                    s, g = groups[G]
                    gb = G % NB

                    def T(jj):
                        i = jj
                        ti = jj % 2
                        k.op("pe", lambda e: [e.transpose(out=tp_ps[ti][:, c * 128:(c + 1) * 128],
                                                          in_=hb[i][:, c * 128:(c + 1) * 128],
                                                          identity=identb[:]) for c in range(8)],
                             R=[b_hb[i], b_const], W=[b_tp[ti]])
                        k.op("act", lambda e: e.copy(
                            out=hT[gb][:, :, jj * 128:(jj + 1) * 128],
                            in_=tp_ps[ti][:].rearrange("p (c t) -> p c t", c=8)), R=[b_tp[ti]],
                             W=([b_hT[gb]] if jj == 0 else []), Wd=([] if jj == 0 else [b_hT[gb]]))

                    def V(jj):
                        j = 4 * g + jj
                        i = jj % NB
                        k.op("pe", lambda e: [e.matmul(
                            v_ps[:], lhsT=hT[gb][:, c, jj * 128:(jj + 1) * 128], rhs=Wbf[:, c, 1024:1536],
                            start=(c == 0), stop=(c == 7)) for c in range(8)] + [e.matmul(
                            vb_ps[:, 0:128], lhsT=hT[gb][:, c, jj * 128:(jj + 1) * 128], rhs=Wbf[:, c, 2176:2304],
                            start=(c == 0), stop=(c == 7)) for c in range(8)],
                             R=[b_hT[gb], b_W], W=[b_vps])
                        k.op("act", lambda e: e.copy(out=vst[i][:, 0:512], in_=v_ps[:]),
                             R=[b_vps], W=[b_vst[i]])
                        k.op("act", lambda e: e.copy(out=vst[i][:, 512:640], in_=vb_ps[:, 0:128]),
                             R=[b_vps], Wd=[b_vst[i]])
                        k.dma("sp", s_vst[i], Vd[s, j * 128:(j + 1) * 128, :], vst[i][:],
                              R=[b_vst[i]], Wd=[b_Vd[s]])
                    T(0)
                    T(1)
                    V(0)
                    T(2)
                    V(1)
                    T(3)
                    V(2)
                    V(3)

                def pairs(G):
                    s, g = groups[G]
                    gb = G % NB
                    for pr in range(13):
                        c0 = PAIRCOL[pr]
                        i = pcount[0] % NB
                        pcount[0] += 1
                        k.op("pe", lambda e, i=i, c0=c0: [e.matmul(
                            x_ps[i][:], lhsT=Wbf[:, c, c0:c0 + 128], rhs=hT[gb][:, c, :],
                            start=(c == 0), stop=(c == 7)) for c in range(8)] + [e.matmul(
                            r_ps[i][:], lhsT=Wrot[:, c, c0:c0 + 128], rhs=hT[gb][:, c, :],
                            start=(c == 0), stop=(c == 7)) for c in range(8)],
                             R=[b_hT[gb], b_W, b_Wrot], W=[b_xps[i]])
                        k.op("dve", lambda e, i=i: e.tensor_tensor(
                            out=t12[i][:, 0, :], in0=x_ps[i][:], in1=cs[gb][:, 0, :], op=ALU.mult),
                             R=[b_xps[i], b_cs[gb]], W=[b_t12[i]])
                        k.op("dve", lambda e, i=i: e.tensor_tensor(
                            out=t12[i][:, 1, :], in0=r_ps[i][:], in1=cs[gb][:, 1, :], op=ALU.mult),
                             R=[b_xps[i], b_cs[gb]], Wd=[b_t12[i]])
                        k.op("pool", lambda e, i=i: e.tensor_tensor(
                            out=qk[i][:], in0=t12[i][:, 0, :], in1=t12[i][:, 1, :], op=ALU.add),
                             R=[b_t12[i]], W=[b_qk[i]])
                        k.dma("sp", s_qk[i], QT[s, pr, :, g * 512:(g + 1) * 512], qk[i][:],
                              R=[b_qk[i]], Wd=[b_QT[s]])

                stA0(0)
                for G in range(len(groups)):
                    stA1(G)
                    pairs(G)
                    if G + 1 < len(groups):
                        stA0(G + 1)
                k.barrier()

            with ExitStack() as ph:
                NCH = S // 128
                vstage = sbt(ph, "vstage", [128, NCH, 256], BF)
                Vaug = [sbt(ph, "Vaug%d" % p, [128, NCH, 4, 65], BF) for p in range(3)]
                Vbaug = sbt(ph, "Vbaug", [128, NCH, 2, 65], BF)
                QTt = [sbt(ph, "QTt%d" % i, [64, S], BF) for i in range(2)]
                KTt = [sbt(ph, "KTt%d" % i, [64, S], BF) for i in range(2)]
                acc = [sbt(ph, "acc%d" % i, [65, S], F32) for i in range(2)]
                KBt = [sbt(ph, "KBt%d" % i, [64, S], BF) for i in range(2)]
                onrm = [sbt(ph, "onrm%d" % i, [64, S], BF) for i in range(2)]
                rdb = sbt(ph, "rdb", [65, S], BF)
                b_rdb = Buf("rdb")
                pT = [sbt(ph, "pT%d" % i, [128, 384], BF) for i in range(6)]
                esink = sbt(ph, "esink", [128, 8], F32)
                s_ps = [pst(ph, "s_ps%d" % i, [128, 512], F32) for i in range(3)]
                o_ps = [pst(ph, "o_ps%d" % i, [65, 512], F32) for i in range(2)]
                bc_ps = [pst(ph, "bc_ps%d" % i, [64, 512], F32) for i in range(2)]
                b_vstage = Buf("vstage")
                b_Vaug = [Buf("Vaug") for _ in range(3)]
                b_Vbaug = Buf("Vbaug")
                b_QTt = [Buf("QTt") for _ in range(2)]
                b_KTt = [Buf("KTt") for _ in range(2)]
                b_acc = [Buf("acc") for _ in range(2)]
                b_KBt = [Buf("KBt") for _ in range(2)]
                b_rden = Buf("rden")
                b_onrm = [Buf("onrm") for _ in range(2)]
                b_pT = [Buf("pT") for _ in range(6)]
                b_esink = Buf("esink")
                b_sps = [Buf("sps") for _ in range(3)]
                b_ops = [Buf("ops") for _ in range(2)]
                b_bc = [Buf("bc") for _ in range(2)]
                s_vstage = k.dsem("s_vstage")
                s_QTt = [k.dsem("s_QTt", _i) for _i in range(2)]
                s_KTt = [k.dsem("s_KTt", _i) for _i in range(2)]
                s_KBt = [k.dsem("s_KBt", _i) for _i in range(2)]
                s_onrm = [k.dsem("s_onrm", _i) for _i in range(2)]
                s_es = k.dsem("s_es")
                cnt = dict(u=0, o=0, bc=0)

                k.dma("sp", s_es, esink[:], sink[l:l + 1, :].partition_broadcast(128), W=[b_esink])
                k.op("act", lambda e: e.activation(out=esink[:], in_=esink[:], func=AF.Exp), R=[b_esink], W=[b_esink])
                for p in range(3):
                    k.op("pool", lambda e, p=p: e.memset(Vaug[p][:], 1.0), W=[b_Vaug[p]])
                k.op("pool", lambda e: e.memset(Vbaug[:], 1.0), W=[b_Vbaug])

                DP = 5
                NSP = 3

                def band_units(units, qv, b_q, kv_, b_k, vfn, b_v, n, w, mask, evac):
                    nblk = (n + 511) // 512
                    for b in range(nblk):
                        q0 = 512 * b
                        q1 = min(n, q0 + 512)
                        blk = dict(oi=None)
                        first = True
                        ulist = []
                        for m in range((n + 127) // 128):
                            base = 128 * m - w
                            lo = max(q0, base, 0)
                            hi = min(q1, 128 * m + 128 + w, n)
                            if hi <= lo:
                                continue
                            ulist.append((m, base, lo, hi, first))
                            first = False
                        for ix, (m, base, lo, hi, first) in enumerate(ulist):
                            N = hi - lo
                            kn = min(128, n - 128 * m)

                            def fS(si, m=m, lo=lo, hi=hi, N=N, kn=kn, base=base):
                                k.op("pe", lambda e: e.matmul(
                                    s_ps[si][0:kn, 0:N], lhsT=kv_(128 * m, 128 * m + kn), rhs=qv(lo, hi),
                                    start=True, stop=True), R=[b_q, b_k], W=[b_sps[si]])

                            def fE(si, pi_, N=N, kn=kn, lo=lo, hi=hi, base=base):
                                k.op("act", lambda e: e.activation(
                                    out=pT[pi_][0:kn, 0:N], in_=s_ps[si][0:kn, 0:N], func=AF.Exp, scale=0.125),
                                     R=[b_sps[si]], W=[b_pT[pi_]])
                                meng = "dve"
                                k.op(meng, lambda e: e.tensor_tensor(
                                    out=pT[pi_][0:kn, 0:N], in0=pT[pi_][0:kn, 0:N],
                                    in1=mask[0:kn, lo - base:hi - base], op=ALU.mult),
                                     R=[b_pT[pi_], b_m01], W=[b_pT[pi_]])

                            def fPV(pi_, m=m, lo=lo, hi=hi, N=N, kn=kn, q0=q0, first=first, blk=blk):
                                if first:
                                    blk["oi"] = cnt["o"] % 2
                                    cnt["o"] += 1
                                oi = blk["oi"]
                                k.op("pe", lambda e: e.matmul(
                                    o_ps[oi][0:65, lo - q0:hi - q0], lhsT=vfn(m)[0:kn, :], rhs=pT[pi_][0:kn, 0:N],
                                    start=first, stop=False, skip_group_check=True),
                                     R=[b_pT[pi_], b_v], W=[b_ops[oi]])

                            post = []
                            if ix == len(ulist) - 1:
                                post.append(lambda b=b, q0=q0, q1=q1, blk=blk: evac(
                                    b, q0, q1, o_ps[blk["oi"]], b_ops[blk["oi"]]))
                            units.append(dict(pre=[], S=fS, E=fE, PV=fPV, post=post))

                deferred = []

                def after_pv():
                    for d_ in deferred:
                        d_[0] -= 1
                    while deferred and deferred[0][0] <= 0:
                        deferred.pop(0)[1]()

                def run_units(units):
                    q = []

                    def pop_pv():
                        v, vi = q.pop(0)
                        v["PV"](vi)
                        for f in v["post"]:
                            f()
                        after_pv()
                    for u in units:
                        if u.get("flush"):
                            while q:
                                pop_pv()
                        for f in u["pre"]:
                            f()
                        ui = cnt["u"]
                        cnt["u"] += 1
                        si = ui % NSP
                        pi_ = ui % (DP + 1)
                        u["S"](si)
                        u["E"](si, pi_)
                        q.append((u, pi_))
                        if len(q) > DP:
                            pop_pv()
                    while q:
                        pop_pv()
                    while deferred:
                        deferred.pop(0)[1]()

                def normalize_store(s, row0, oi, ai):
                    k.op("act", lambda e: e.activation(out=acc[ai][64:65, :], in_=acc[ai][64:65, :], func=AF.Ln),
                         R=[b_acc[ai]], W=[b_acc[ai]])
                    k.op("act", lambda e: e.activation(out=rdb[64:65, :], in_=acc[ai][64:65, :], func=AF.Exp,
                                                       scale=-1.0), R=[b_acc[ai]], W=[b_rdb])

                    def part2():
                        for g in range(NG):
                            bi = cnt["bc"] % 2
                            cnt["bc"] += 1
                            k.op("pe", lambda e, g=g, bi=bi: e.matmul(
                                bc_ps[bi][:], lhsT=onesbp[64:65, 0:64], rhs=rdb[64:65, g * 512:(g + 1) * 512],
                                start=True, stop=True), R=[b_rdb, b_const], W=[b_bc[bi]])
                            k.op("dve", lambda e, g=g, bi=bi: e.tensor_tensor(
                                out=onrm[oi][:, g * 512:(g + 1) * 512], in0=bc_ps[bi][:],
                                in1=acc[ai][0:64, g * 512:(g + 1) * 512], op=ALU.mult),
                                 R=[b_bc[bi], b_acc[ai]], Wd=[b_onrm[oi]])
                        k.dma("sp", s_onrm[oi], mixT[s, row0:row0 + 64, :], onrm[oi][:], R=[b_onrm[oi]],
                              Wd=[b_mixT[s]])
                    deferred.append([10, part2])

                hcount = 0
                for s in range(NS):
                    units = []
                    heads = []
                    for hg in range(2):
                        def load_v(s=s, hg=hg):
                            for p, d in enumerate(PATTERNS):
                                n = S // d
                                nch = n // 128
                                vsrc = Vd[s].rearrange("(m k r) c -> k r m c", k=128, r=d)
                                for r in range(d):
                                    k.dma("sp", s_vstage, vstage[:, r * nch:(r + 1) * nch, :],
                                          vsrc[:, r, :, hg * 256:(hg + 1) * 256],
                                          R=[b_Vd[s]], W=([b_vstage] if r == 0 else []), Wd=([] if r == 0 else [b_vstage]))
                                for hh in range(4):
                                    k.op("dve" if hh % 2 == 0 else "pool", lambda e, p=p, hh=hh: e.tensor_copy(
                                        out=Vaug[p][:, :, hh, 0:64], in_=vstage[:, :, hh * 64:(hh + 1) * 64]),
                                         R=[b_vstage], W=([b_Vaug[p]] if hh == 0 else []), Wd=([] if hh == 0 else [b_Vaug[p]]))
                        for hh in range(4):
                            h = hg * 4 + hh
                            qi = hcount % 2
                            hcount += 1

                            def load_qk(s=s, h=h, qi=qi):
                                k.dma("sp", s_QTt[qi], QTt[qi][:], QT[s, h // 2, (h % 2) * 64:(h % 2) * 64 + 64, :],
                                      R=[b_QT[s]], W=[b_QTt[qi]])
                                k.dma("sp", s_KTt[qi], KTt[qi][:], QT[s, 4 + h // 2, (h % 2) * 64:(h % 2) * 64 + 64, :],
                                      R=[b_QT[s]], W=[b_KTt[qi]])
                            u0 = len(units)
                            for p, d in enumerate(PATTERNS):
                                n = S // d
                                nch = n // 128
                                for r in range(d):
                                    def qv(lo, hi, r=r, d=d, qi=qi):
                                        return QTt[qi][:, r + d * lo:r + d * (hi - 1) + 1:d]

                                    def kv_(lo, hi, r=r, d=d, qi=qi):
                                        return KTt[qi][:, r + d * lo:r + d * (hi - 1) + 1:d]

                                    def vfn(m, p=p, r=r, nch=nch, hh=hh):
                                        return Vaug[p][:, r * nch + m, hh, :]

                                    def evac(b, q0, q1, ops, b_o, p=p, r=r, d=d, qi=qi):
                                        dst = acc[qi][:, r + d * q0:r + d * (q1 - 1) + 1:d]
                                        if p == 0:
                                            k.op("act", lambda e: e.copy(out=dst, in_=ops[0:65, 0:q1 - q0]),
                                                 R=[b_o], Wd=[b_acc[qi]])
                                        else:
                                            k.op("dve", lambda e: e.tensor_tensor(
                                                out=dst, in0=ops[0:65, 0:q1 - q0], in1=dst, op=ALU.add),
                                                 R=[b_o, b_acc[qi]], W=[b_acc[qi]])
                                    band_units(units, qv, b_QTt[qi], kv_, b_KTt[qi], vfn, b_Vaug[p], n, 64, mA01, evac)
                            if hh == 0:
                                units[u0]["pre"].append(load_v)
                                units[u0]["flush"] = True
                            heads.append((u0, load_qk))
                            units[-1]["post"].append(lambda s=s, h=h, qi=qi: normalize_store(s, h * 64, qi, qi))
                    def load_vb(s=s):
                        vsrc = Vd[s].rearrange("(m k) c -> k m c", k=128)
                        k.dma("sp", s_vstage, vstage[:, :, 0:128], vsrc[:, :, 512:640], R=[b_Vd[s]], W=[b_vstage])
                        for kvh in range(2):
                            k.op("dve", lambda e, kvh=kvh: e.tensor_copy(
                                out=Vbaug[:, :, kvh, 0:64], in_=vstage[:, :, kvh * 64:(kvh + 1) * 64]),
                                 R=[b_vstage], W=([b_Vbaug] if kvh == 0 else []), Wd=([] if kvh == 0 else [b_Vbaug]))
                    for kvh in range(2):
                        ki = kvh % 2
                        for g4 in range(4):
                            h = kvh * 4 + g4
                            qi = hcount % 2
                            hcount += 1

                            def load_qk(s=s, h=h, qi=qi, kvh=kvh, ki=ki, g4=g4):
                                if g4 == 0:
                                    k.dma("sp", s_KBt[ki], KBt[ki][:], QT[s, 12, kvh * 64:kvh * 64 + 64, :],
                                          R=[b_QT[s]], W=[b_KBt[ki]])
                                k.dma("sp", s_QTt[qi], QTt[qi][:], QT[s, 8 + h // 2, (h % 2) * 64:(h % 2) * 64 + 64, :],
                                      R=[b_QT[s]], W=[b_QTt[qi]])

                            def qv(lo, hi, qi=qi):
                                return QTt[qi][:, lo:hi]

                            def kv_(lo, hi, ki=ki):
                                return KBt[ki][:, lo:hi]

                            def vfn(m, kvh=kvh):
                                return Vbaug[:, m, kvh, :]

                            def evac(b, q0, q1, ops, b_o, qi=qi):
                                k.op("act", lambda e: e.copy(out=acc[qi][:, q0:q1], in_=ops[0:65, 0:q1 - q0]),
                                     R=[b_o], Wd=[b_acc[qi]])
                            u0 = len(units)
                            band_units(units, qv, b_QTt[qi], kv_, b_KBt[ki], vfn, b_Vbaug, S, 128, mB01, evac)
                            if kvh == 0 and g4 == 0:
                                units[u0]["pre"].append(load_vb)
                                units[u0]["flush"] = True
                            heads.append((u0, load_qk))

                            def fin(s=s, h=h, qi=qi):
                                k.op("dve", lambda e: e.tensor_scalar(
                                    out=acc[qi][64:65, :], in0=acc[qi][64:65, :], scalar1=esink[64:65, h:h + 1],
                                    scalar2=None, op0=ALU.add), R=[b_acc[qi], b_esink], W=[b_acc[qi]])
                                normalize_store(s, 512 + h * 64, qi, qi)
                            units[-1]["post"].append(fin)
                    for hi_, (u0_, lq) in enumerate(heads):
                        if hi_ == 0:
                            units[u0_]["pre"].insert(0, lq)
                        else:
                            units[heads[hi_ - 1][0]]["pre"].append(lq)
                    run_units(units)
                k.barrier()

            with ExitStack() as ph:
                Wof = sbt(ph, "Wof", [128, 8, D], F32)
                Wo = sbt(ph, "Wo", [128, 8, D], BF)
                gm = sbt(ph, "gm", [128, 8], F32)
                gF = sbt(ph, "gF", [128, D], F32)
                wr = sbt(ph, "wr", [128, 8, NE], F32)
                b_Wof = Buf("Wof")
                b_Wo = Buf("Wo")
                b_gm = Buf("gm")
                b_gF = Buf("gF")
                b_wr = Buf("wr")
                wsem = k.dsem("wsemC")
                k.dma("sp", wsem, Wof[:], w_out[l].rearrange("(c p) n -> p c n", p=128), W=[b_Wof])
                for c in range(8):
                    k.dma("sp", k.dsem("gmsem"), gm[:, c:c + 1], g_mix[l, c * 128:(c + 1) * 128].rearrange("(p o) -> p o", o=1),
                          Wd=[b_gm])
                k.dma("sp", k.dsem("gFsem"), gF[:], g_ffn[l:l + 1, :].partition_broadcast(128), W=[b_gF])
                k.dma("sp", k.dsem("wrsem"), wr[:], w_router[l].rearrange("(c p) n -> p c n", p=128), W=[b_wr])
                for c in range(8):
                    k.op("dve" if c % 2 == 0 else "pool", lambda e, c=c: e.tensor_scalar(
                        out=Wo[:, c, :], in0=Wof[:, c, :], scalar1=gm[:, c:c + 1], scalar2=None, op0=ALU.mult),
                         R=[b_Wof, b_gm], Wd=[b_Wo])

                NB = 2
                NT3 = 3
                mT = [sbt(ph, "mT%d" % i, [128, 8, 512], BF) for i in range(NB)]
                sq = [sbt(ph, "sq%d" % i, [128, 8, 512], BF) for i in range(NB)]
                onesb = sbt(ph, "onesb", [128, 2], BF)
                NBIG = 3
                NSM = 6
                xt = [sbt(ph, "xtC%d" % i, [128, D], F32) for i in range(NBIG)]
                tmp = [sbt(ph, "tmpC%d" % i, [128, D], F32) for i in range(2)]
                xn = [sbt(ph, "xnC%d" % i, [128, D], F32) for i in range(NBIG)]
                junk = [sbt(ph, "junkC%d" % i, [128, D], BF) for i in range(2)]
                h2f = [sbt(ph, "h2f%d" % i, [128, D], F32) for i in range(NBIG)]
                h2b = [sbt(ph, "h2b%d" % i, [128, D], BF) for i in range(NBIG)]
                h2T = [sbt(ph, "h2T%d" % i, [128, 8, 128], F32) for i in range(NBIG)]
                stc = [sbt(ph, "stc%d" % i, [128, 16], F32) for i in range(NSM)]
                lge = [sbt(ph, "lge%d" % i, [128, 2, NE], F32) for i in range(NSM)]
                ssq_ps = pst(ph, "ssq_ps", [128, 512], F32)
                ya_ps = pst(ph, "ya_ps", [128, 1024], F32)
                yb_ps = pst(ph, "yb_ps", [128, 512], F32)
                tpf_ps = pst(ph, "tpf_ps", [128, 8, 128], F32)
                lg_ps = pst(ph, "lg_ps", [128, 512], F32)
                at_ps = pst(ph, "at_ps", [NE, 512], F32)
                b_mT = [Buf("mT") for _ in range(NB)]
                b_sq = [Buf("sq") for _ in range(NB)]
                b_onesb = Buf("onesb")
                b_xt = [Buf("xtC") for _ in range(NBIG)]
                b_tmp = [Buf("tmp") for _ in range(2)]
                b_xn = [Buf("xn") for _ in range(NBIG)]
                b_junk = [Buf("junk") for _ in range(2)]
                b_h2f = [Buf("h2f") for _ in range(NBIG)]
                b_h2b = [Buf("h2b") for _ in range(NBIG)]
                b_h2T = [Buf("h2T") for _ in range(NBIG)]
                b_stc = [Buf("stc") for _ in range(NSM)]
                b_stn = [Buf("stn") for _ in range(NSM)]
                b_lge = [Buf("lge") for _ in range(NSM)]
                b_ssq = Buf("ssqps")
                b_ya = Buf("ya")
                b_yb = Buf("yb")
                b_tpf = Buf("tpf")
                b_lg = Buf("lg")
                b_at = Buf("at")
                s_mT = [k.dsem("s_mT", _i) for _i in range(NB)]
                s_xt = [k.dsem("s_xtC", _i) for _i in range(NBIG)]
                s_xn = [k.dsem("s_xn", _i) for _i in range(NBIG)]
                s_h2b = [k.dsem("s_h2b", _i) for _i in range(NBIG)]
                k.op("dve", lambda e: e.memset(onesb[:], 1.0), W=[b_onesb])

                groups = [(s, g) for s in range(NS) for g in range(NG)]
                tiles = [(Gi, jj) for Gi in range(len(groups)) for jj in range(4)]

                def grp_pro(Gi):
                    s, g = groups[Gi]
                    gb = Gi % NB
                    k.dma("sp", s_mT[gb], mT[gb][:],
                          mixT[s].rearrange("(c p) t -> p c t", p=128)[:, :, g * 512:(g + 1) * 512],
                          R=[b_mixT[s]], W=[b_mT[gb]])
                    k.op("act", lambda e: e.activation(out=sq[gb][:], in_=mT[gb][:], func=AF.Square),
                         R=[b_mT[gb]], W=[b_sq[gb]])

                def st0(t):
                    Gi, jj = tiles[t]
                    s, g = groups[Gi]
                    gb = Gi % NB
                    j = 4 * g + jj
                    i = t % NBIG
                    m_ = t % NSM
                    cols = slice(jj * 128, (jj + 1) * 128)
                    k.dma("act", s_xt[i], xt[i][:], xsrc[s][j * 128:(j + 1) * 128, :],
                          R=([b_xsrc[s]] if b_xsrc else []), W=[b_xt[i]])
                    k.op("pe", lambda e: [e.matmul(
                        ssq_ps[:, 0:1], lhsT=sq[gb][:, c, cols], rhs=onesb[:, 0:1],
                        start=(c == 0), stop=(c == 3)) for c in range(4)] + [e.matmul(
                        ssq_ps[:, 1:2], lhsT=sq[gb][:, c, cols], rhs=onesb[:, 0:1],
                        start=False, stop=(c == 7), skip_group_check=True) for c in range(4, 8)],
                         R=[b_sq[gb], b_onesb], W=[b_ssq])
                    k.op("act", lambda e: e.activation(
                        out=stc[m_][:, 0:2], in_=ssq_ps[:, 0:2], func=AF.Sqrt, bias=epst[:, 0:1],
                        scale=1.0 / 512), R=[b_ssq, b_const], W=[b_stc[m_]])
                    k.op("dve", lambda e: e.reciprocal(out=stc[m_][:, 2:4], in_=stc[m_][:, 0:2]),
                         R=[b_stc[m_]], W=[b_stc[m_]])

                def st1(t):
                    Gi, jj = tiles[t]
                    s, g = groups[Gi]
                    gb = Gi % NB
                    j = 4 * g + jj
                    i = t % NBIG
                    i2 = t % 2
                    m_ = t % NSM
                    cols = slice(jj * 128, (jj + 1) * 128)
                    k.op("pe", lambda e: [e.matmul(
                        ya_ps[:, half * 512:(half + 1) * 512], lhsT=mT[gb][:, c, cols],
                        rhs=Wo[:, c, half * 512:(half + 1) * 512],
                        start=(c == 0), stop=(c == 3)) for half in range(2) for c in range(4)],
                         R=[b_mT[gb], b_Wo], W=[b_ya])
                    k.op("dve", lambda e: e.scalar_tensor_tensor(
                        out=tmp[i2][:], in0=ya_ps[:], scalar=stc[m_][:, 2:3], in1=xt[i][:],
                        op0=ALU.mult, op1=ALU.add), R=[b_ya, b_stc[m_], b_xt[i]], W=[b_tmp[i2]])
                    for half in range(2):
                        hs = slice(half * 512, (half + 1) * 512)
                        k.op("pe", lambda e, hs=hs: [e.matmul(
                            yb_ps[:], lhsT=mT[gb][:, c, cols], rhs=Wo[:, c, hs],
                            start=(c == 4), stop=(c == 7)) for c in range(4, 8)],
                             R=[b_mT[gb], b_Wo], W=[b_yb])
                        k.op("dve", lambda e, hs=hs: e.scalar_tensor_tensor(
                            out=xn[i][:, hs], in0=yb_ps[:], scalar=stc[m_][:, 3:4], in1=tmp[i2][:, hs],
                            op0=ALU.mult, op1=ALU.add), R=[b_yb, b_stc[m_], b_tmp[i2]],
                             W=([b_xn[i]] if half == 0 else []), Wd=([] if half == 0 else [b_xn[i]]))
                    k.dma("sp", s_xn[i], xr[s][j * 128:(j + 1) * 128, :], xn[i][:], R=[b_xn[i]], Wd=[b_xr[s]])
                    rms_rstd(None, xn[i][:], b_xn[i], D, stc[m_][:, 4:5], stc[m_][:, 5:6], stc[m_][:, 6:7],
                             junk[i2][:], b_stn[m_])
                    k.op("dve", lambda e: e.scalar_tensor_tensor(
                        out=h2f[i][:], in0=xn[i][:], scalar=stc[m_][:, 6:7], in1=gF[:],
                        op0=ALU.mult, op1=ALU.mult), R=[b_xn[i], b_stn[m_], b_gF], W=[b_h2f[i]])
                    k.op("pool", lambda e: e.tensor_copy(out=h2b[i][:], in_=h2f[i][:]),
                         R=[b_h2f[i]], W=[b_h2b[i]])
                    k.dma("sp", s_h2b[i], h2d[s][j * 128:(j + 1) * 128, :], h2b[i][:],
                          R=[b_h2b[i]], Wd=[b_h2d[s]])

                def st2a(t):
                    i = t % NBIG
                    k.op("pe", lambda e: [e.transpose(out=tpf_ps[:, c, :], in_=h2f[i][:, c * 128:(c + 1) * 128],
                                                      identity=identf[:]) for c in range(8)],
                         R=[b_h2f[i], b_const], W=[b_tpf])
                    k.op("act", lambda e: e.copy(out=h2T[i][:], in_=tpf_ps[:]), R=[b_tpf], W=[b_h2T[i]])

                def st2b(t):
                    i = t % NBIG
                    m_ = t % NSM
                    k.op("pe", lambda e: [e.matmul(lg_ps[:, 0:NE], lhsT=h2T[i][:, c, :], rhs=wr[:, c, :],
                                                   start=(c == 0), stop=(c == 7)) for c in range(8)],
                         R=[b_h2T[i], b_wr], W=[b_lg])
                    k.op("dve", lambda e: e.tensor_reduce(out=stc[m_][:, 8:9], in_=lg_ps[:, 0:NE], axis=AX.X,
                                                          op=ALU.max, negate=True),
                         R=[b_lg], W=[b_lge[m_]])
                    k.op("act", lambda e: e.activation(out=lge[m_][:, 0, :], in_=lg_ps[:, 0:NE], func=AF.Exp,
                                                       bias=stc[m_][:, 8:9], accum_out=stc[m_][:, 9:10]),
                         R=[b_lg, b_lge[m_]], W=[b_lge[m_]])
                    k.op("dve", lambda e: e.reciprocal(out=stc[m_][:, 10:11], in_=stc[m_][:, 9:10]),
                         R=[b_lge[m_]], W=[b_lge[m_]])
                    k.op("dve", lambda e: e.tensor_scalar(
                        out=lge[m_][:, 1, :], in0=lge[m_][:, 0, :], scalar1=stc[m_][:, 10:11], scalar2=None,
                        op0=ALU.mult), R=[b_lge[m_]], W=[b_lge[m_]])

                def st2c(t):
                    Gi, jj = tiles[t]
                    s, g = groups[Gi]
                    m_ = t % NSM
                    cols = slice(jj * 128, (jj + 1) * 128)
                    k.op("pe", lambda e: e.transpose(
                        out=at_ps[:, cols], in_=lge[m_][:, 1, :], identity=identf[:]),
                         R=[b_lge[m_], b_const], Wd=[b_at])
                    if jj == 3:
                        k.op("act", lambda e: e.copy(
                            out=affT[s * 32:s * 32 + NE, g * 512:(g + 1) * 512], in_=at_ps[:]),
                             R=[b_at], Wd=[b_affT])

                grp_pro(0)
                NTl = len(tiles)
                stages = [st0, st1, st2a, st2b, st2c]
                for step in range(NTl + len(stages) - 1):
                    for si_, stf_ in enumerate(stages):
                        t = step - si_
                        if 0 <= t < NTl:
                            stf_(t)
                    if step < NTl and tiles[step][1] == 1 and tiles[step][0] + 1 < len(groups):
                        grp_pro(tiles[step][0] + 1)
                k.barrier()

            with ExitStack() as ph:
                NP = 32 * (NS - 1) + NE
                work = sbt(ph, "work", [NP, S], F32)
                vals = sbt(ph, "vals", [NP, C], F32)
                idxu = sbt(ph, "idxu", [NP, C], U32)
                idxf = sbt(ph, "idxf", [NP, C], F32)
                tpi_ps = pst(ph, "tpi_ps", [128, 512], F32)[:, 0:NCC * 48].rearrange("p (c n) -> p c n", n=48)
                tpv_ps = pst(ph, "tpv_ps", [128, 512], F32)[:, 0:NCC * 48].rearrange("p (c n) -> p c n", n=48)
                b_work = Buf("work")
                b_vals = Buf("vals")
                b_idxu = Buf("idxu")
                b_idxf = Buf("idxf")
                b_tpi = Buf("tpi")
                k.op("dve", lambda e: e.tensor_copy(out=work[:], in_=affT[0:NP, :]), R=[b_affT], W=[b_work])
                for it in range(C // 8):
                    sl = slice(it * 8, it * 8 + 8)
                    k.op("dve", lambda e, sl=sl: e.max(out=vals[:, sl], in_=work[:]), R=[b_work], Wd=[b_vals])
                    k.op("dve", lambda e, sl=sl: e.max_index(out=idxu[:, sl], in_max=vals[:, sl], in_values=work[:]),
                         R=[b_work, b_vals], Wd=[b_idxu])
                    k.op("dve", lambda e, sl=sl: e.match_replace(out=work[:], in_to_replace=vals[:, sl],
                                                                 in_values=work[:], imm_value=-1.0),
                         R=[b_vals, b_idxu], W=[b_work])
                k.op("dve", lambda e: e.tensor_copy(out=idxf[:], in_=idxu[:]), R=[b_idxu], W=[b_idxf])
                k.op("pe", lambda e: [e.transpose(out=tpi_ps[:, cc, 0:NP], in_=idxf[:, cc * 128:(cc + 1) * 128],
                                                  identity=identf[0:NP, 0:NP]) for cc in range(NCC)] +
                     [e.transpose(out=tpv_ps[:, cc, 0:NP], in_=vals[:, cc * 128:(cc + 1) * 128],
                                  identity=identf[0:NP, 0:NP]) for cc in range(NCC)],
                     R=[b_idxf, b_vals, b_const], W=[b_tpi])
                k.op("dve", lambda e: e.tensor_copy(out=idxT[:, :, 0:NP], in_=tpi_ps[:, :, 0:NP]), R=[b_tpi], W=[b_idxT])
                k.op("act", lambda e: e.copy(out=gateT[:, :, 0:NP], in_=tpv_ps[:, :, 0:NP]), R=[b_tpi], W=[b_gateT])
                if dbg and l == 0:
                    dsm = k.dsem("dbgsem")
                    k.dma("sp", dsm, dbg_idx, idxT[:], R=[b_idxT])
                    k.dma("sp", dsm, dbg_gate, gateT[:], R=[b_gateT])
                    k.dma("sp", dsm, dbg_aff, affT[:], R=[b_affT])
                k.barrier()

            with ExitStack() as ph:
              if "E" not in skip:
                  NWB = 2
                  Wg = [sbt(ph, "Wg%d" % i, [128, 8, D], BF) for i in range(NWB)]
                  Wu = [sbt(ph, "Wu%d" % i, [128, 8, D], BF) for i in range(NWB)]
                  Wd_ = [sbt(ph, "Wd%d" % i, [128, 8, D], BF) for i in range(NWB)]
                  NXE = 4
                  NYE = 4
                  xe = [sbt(ph, "xe%d" % i, [128, NCC, D], BF) for i in range(NXE)]
                  xeT = [sbt(ph, "xeT%d" % i, [128, 8, C], BF) for i in range(2)]
                  sg = [sbt(ph, "sg%d" % i, [128, C], F32) for i in range(2)]
                  hidT = [sbt(ph, "hidT%d" % i, [128, 8, C], BF) for i in range(2)]
                  ye = [sbt(ph, "ye%d" % i, [128, D], F32) for i in range(NYE)]
                  tpe_ps = [pst(ph, "tpe_ps%d" % i, [128, 1024], BF) for i in range(2)]
                  g_ps = [pst(ph, "g_ps%d" % i, [128, 512], F32) for i in range(2)]
                  u_ps = [pst(ph, "u_ps%d" % i, [128, 512], F32) for i in range(2)]
                  y_ps = [pst(ph, "y_ps%d" % i, [128, 512], F32) for i in range(2)]
                  b_Wg = [Buf("Wg") for _ in range(NWB)]
                  b_xe = [Buf("xe") for _ in range(NXE)]
                  b_xeT = [Buf("xeT") for _ in range(2)]
                  b_sg = [Buf("sg") for _ in range(2)]
                  b_hidT = [Buf("hidT") for _ in range(2)]
                  b_ye = [Buf("ye") for _ in range(NYE)]
                  b_tpe = [Buf("tpe") for _ in range(2)]
                  b_gu = [Buf("gu") for _ in range(2)]
                  b_yps = [Buf("yps") for _ in range(2)]
                  s_W = [k.dsem("s_W", _i) for _i in range(NWB)]
                  s_xe = [k.dsem("s_xe", _i) for _i in range(NXE)]
                  s_ye = [k.dsem("s_ye", _i) for _i in range(NYE)]
                  cnt = dict(xe=0, tp=0, gu=0, y=0, ye=0)

                  def load_w(e_):
                      wi = e_ % NWB
                      for (wt, src) in ((Wg[wi], w_gate), (Wu[wi], w_up), (Wd_[wi], w_down)):
                          k.dma("pool", s_W[wi], wt[:], src[l, e_].rearrange("(c p) n -> p c n", p=128),
                                W=([b_Wg[wi]] if wt is Wg[wi] else []), Wd=([] if wt is Wg[wi] else [b_Wg[wi]]))

                  bc_reg = nc.gpsimd.to_reg(S - 1)
                  items = [(e_, s) for e_ in range(NE) for s in range(NS)]

                  def gather(ii):
                      e_, s = items[ii]
                      col = s * 32 + e_
                      xi = ii % NXE
                      for cc in range(NCC):
                          k.idma(s_xe[xi], out=xe[xi][:, cc, :], out_offset=None, in_=h2d[s],
                                 in_offset=bass.IndirectOffsetOnAxis(ap=idxT[:, cc, col:col + 1], axis=0),
                                 R=[b_h2d[s], b_idxT], W=([b_xe[xi]] if cc == 0 else []),
                                 Wd=([] if cc == 0 else [b_xe[xi]]))

                  def tr_(ii):
                      e_, s = items[ii]
                      wi = e_ % NWB
                      col = s * 32 + e_
                      xi = ii % NXE
                      hi_ = ii % 2
                      for c in range(8):
                          ti = cnt["tp"] % 2
                          cnt["tp"] += 1
                          k.op("pe", lambda e, c=c, ti=ti: [e.transpose(
                              out=tpe_ps[ti][:, cc * 128:(cc + 1) * 128], in_=xe[xi][:, cc, c * 128:(c + 1) * 128],
                              identity=identb[:]) for cc in range(NCC)], R=[b_xe[xi], b_const], W=[b_tpe[ti]])
                          k.op("act" if c % 2 == 0 else "dve", lambda e, c=c, ti=ti: (
                              e.copy(out=xeT[hi_][:, c, :], in_=tpe_ps[ti][:, 0:C]) if c % 2 == 0 else
                              e.tensor_copy(out=xeT[hi_][:, c, :], in_=tpe_ps[ti][:, 0:C])),
                               R=[b_tpe[ti]], W=([b_xeT[hi_]] if c == 0 else []), Wd=([] if c == 0 else [b_xeT[hi_]]))

                  def gu_(ii):
                      e_, s = items[ii]
                      wi = e_ % NWB
                      col = s * 32 + e_
                      xi = ii % NXE
                      hi_ = ii % 2
                      for fc in range(8):
                          gi = cnt["gu"] % 2
                          cnt["gu"] += 1
                          fs = slice(fc * 128, (fc + 1) * 128)
                          k.op("pe", lambda e, fs=fs, gi=gi: [e.matmul(
                              g_ps[gi][:, 0:C], lhsT=Wg[wi][:, c, fs], rhs=xeT[hi_][:, c, :], start=(c == 0), stop=(c == 7))
                              for c in range(8)] + [e.matmul(
                              u_ps[gi][:, 0:C], lhsT=Wu[wi][:, c, fs], rhs=xeT[hi_][:, c, :], start=(c == 0), stop=(c == 7))
                              for c in range(8)], R=[b_xeT[hi_], b_Wg[wi]], W=[b_gu[gi]])
                          k.op("act", lambda e, gi=gi: e.activation(out=sg[gi][:], in_=g_ps[gi][:, 0:C], func=AF.Silu),
                               R=[b_gu[gi]], W=[b_sg[gi]])
                          k.op("dve", lambda e, gi=gi, fc=fc: e.tensor_tensor(
                              out=hidT[hi_][:, fc, :], in0=u_ps[gi][:, 0:C], in1=sg[gi][:], op=ALU.mult),
                               R=[b_gu[gi], b_sg[gi]], W=([b_hidT[hi_]] if fc == 0 else []),
                               Wd=([] if fc == 0 else [b_hidT[hi_]]))

                  def dn_(ii):
                      e_, s = items[ii]
                      wi = e_ % NWB
                      col = s * 32 + e_
                      xi = ii % NXE
                      hi_ = ii % 2
                      for cc in range(NCC):
                          yi = cnt["ye"] % NYE
                          cnt["ye"] += 1
                          for half in range(2):
                              pi_ = cnt["y"] % 2
                              cnt["y"] += 1
                              hs = slice(half * 512, (half + 1) * 512)
                              k.op("pe", lambda e, cc=cc, hs=hs, pi_=pi_: [e.matmul(
                                  y_ps[pi_][:], lhsT=hidT[hi_][:, fc, cc * 128:(cc + 1) * 128], rhs=Wd_[wi][:, fc, hs],
                                  start=(fc == 0), stop=(fc == 7)) for fc in range(8)],
                                   R=[b_hidT[hi_], b_Wg[wi]], W=[b_yps[pi_]])
                              k.op("act", lambda e, cc=cc, hs=hs, pi_=pi_, yi=yi: e.activation(
                                  out=ye[yi][:, hs], in_=y_ps[pi_][:], func=AF.Copy,
                                  scale=gateT[:, cc, col:col + 1]), R=[b_yps[pi_], b_gateT],
                                   W=([b_ye[yi]] if half == 0 else []), Wd=([] if half == 0 else [b_ye[yi]]))
                          k.idma(s_ye[yi], out=xr[s], out_offset=bass.IndirectOffsetOnAxis(
                              ap=idxT[:, cc, col:col + 1], axis=0), in_=ye[yi][:], in_offset=None,
                                 compute_op=ALU.add, bounds_check=bc_reg, oob_is_err=True,
                                 R=[b_ye[yi], b_idxT], W=([b_xr[s]] if cc == 0 else []),
                                 Wd=([] if cc == 0 else [b_xr[s]]))


                  load_w(0)
                  for ii in range(min(2, len(items))):
                      gather(ii)
                  tr_(0)
                  for ii, (e_, s) in enumerate(items):
                      if ii + 2 < len(items):
                          gather(ii + 2)
                      if s == 0 and e_ + 1 < NE:
                          load_w(e_ + 1)
                      gu_(ii)
                      if ii + 1 < len(items):
                          tr_(ii + 1)
                      dn_(ii)
                  k.barrier()

        with ExitStack() as ph:
            gL = sbt(ph, "gL", [128, D], F32)
            b_gL = Buf("gL")
            fsem = k.dsem("fsem")
            k.dma("sp", fsem, gL[:], g_final[0:1, :].partition_broadcast(128), W=[b_gL])
            NB = 4
            xt = [sbt(ph, "xtF%d" % i, [128, D], F32) for i in range(NB)]
            yo = [sbt(ph, "yoF%d" % i, [128, D], F32) for i in range(NB)]
            junk = [sbt(ph, "junkF%d" % i, [128, D], BF) for i in range(NB)]
            stf = [sbt(ph, "stf%d" % i, [128, 4], F32) for i in range(NB)]
            b_xt = [Buf("xtF") for _ in range(NB)]
            b_yo = [Buf("yoF") for _ in range(NB)]
            b_st = [Buf("stF") for _ in range(NB)]
            s_xt = [k.dsem("s_xtF", _i) for _i in range(NB)]
            s_yo = [k.dsem("s_yoF", _i) for _i in range(NB)]
            ti = 0
            for s in range(NS):
                for j in range(NT):
                    i = ti % NB
                    ti += 1
                    k.dma("act", s_xt[i], xt[i][:], xr[s][j * 128:(j + 1) * 128, :], R=[b_xr[s]], W=[b_xt[i]])
                    rms_rstd(None, xt[i][:], b_xt[i], D, stf[i][:, 0:1], stf[i][:, 1:2], stf[i][:, 2:3],
                             junk[i][:], b_st[i])
                    k.op("dve", lambda e, i=i: e.scalar_tensor_tensor(
                        out=yo[i][:], in0=xt[i][:], scalar=stf[i][:, 2:3], in1=gL[:], op0=ALU.mult, op1=ALU.mult),
                         R=[b_xt[i], b_st[i], b_gL], W=[b_yo[i]])
                    k.dma("sp", s_yo[i], y_out[s, j * 128:(j + 1) * 128, :], yo[i][:], R=[b_yo[i]], Wd=[b_y])
            k.barrier()
    return nc


def rope_tables_T(S):
    inv = (1.0 / (np.float32(10000.0) ** (np.arange(0, HD, 2, dtype=np.float32) / np.float32(HD)))).astype(np.float32)
    ang = (np.arange(S, dtype=np.float32)[None, :] * inv[:, None]).astype(np.float32)
    cosT = np.tile(np.cos(ang).astype(np.float32), (4, 1))
    sinT = np.tile(np.sin(ang).astype(np.float32), (4, 1))
    return np.ascontiguousarray(cosT), np.ascontiguousarray(sinT)


def band_mask(w):
    kap = np.arange(128)[:, None]
    th = np.arange(128 + 2 * w)[None, :]
    valid = (kap <= th) & (th <= kap + 2 * w)
    return np.where(valid, 0.0, NEG).astype(ml_dtypes.bfloat16)


def const_inputs(S):
    cosT, sinT = rope_tables_T(S)
    return dict(cosT=cosT, sinT=sinT,
                ident_bf=np.eye(128, dtype=np.float32).astype(ml_dtypes.bfloat16),
                ident_f=np.eye(128, dtype=np.float32),
                maskA=band_mask(64), maskB=band_mask(128))


def make_in_maps(inputs, n_cores, NS):
    f = lambda a: np.ascontiguousarray(np.asarray(a, dtype=np.float32))
    x = f(inputs["x"])
    S = x.shape[1]
    L = inputs["w_in"].shape[0]
    shared = dict(
        w_in=f(inputs["w_in"]), w_out=f(inputs["w_out"]), g_attn=f(inputs["g_attn"]),
        g_mix=np.ascontiguousarray(np.concatenate([f(inputs["g_mix_a"]), f(inputs["g_mix_b"])], axis=1)),
        sink=f(inputs["sink"]).reshape(L, 8), g_ffn=f(inputs["g_ffn"]), w_router=f(inputs["w_router"]),
        w_gate=f(inputs["w_gate"]), w_up=f(inputs["w_up"]), w_down=f(inputs["w_down"]),
        g_final=f(inputs["g_final"]).reshape(1, D))
    shared.update(const_inputs(S))
    maps = []
    for c in range(n_cores):
        m = dict(shared)
        m["x"] = np.ascontiguousarray(x[c * NS:(c + 1) * NS])
        maps.append(m)
    return maps


def kernel(**inputs):
    x = np.asarray(inputs["x"])
    B, S, _ = x.shape
    L = np.asarray(inputs["w_in"]).shape[0]
    n_cores = 8
    NS = B // n_cores
    nc = build_program(S, NS, L)
    maps = make_in_maps(inputs, n_cores, NS)
    res = run_bass_kernel_spmd(nc, maps, core_ids=list(range(n_cores)))
    out = np.concatenate([np.asarray(r["y"]) for r in res.results], axis=0)
    return out.astype(np.float32)
```
